# Optimizing a Trainium2 kernel written in Bass

```python
import math
import jax, jax.numpy as jnp
from jax import lax
import numpy as np

D_MODEL = 1024
BATCH = 16
SEQ = 2048
DEPTH = 2

N_MIXERS = 2
N_LAYERS_A = (DEPTH + 1) // 2
N_LAYERS_B = DEPTH // 2

A_HEADS = 4
A_QK_DIM = D_MODEL // (2 * A_HEADS)
A_V_DIM = D_MODEL // A_HEADS
A_CHUNK = 128
A_HQ = A_HEADS * A_QK_DIM
A_HV = A_HEADS * A_V_DIM
A_IN_COLS = 2 * A_HQ + 2 * A_HV + 2 * A_HEADS

B_HEADS = 8
B_HEAD_DIM = D_MODEL // (2 * B_HEADS)
B_V_DIM = 2 * B_HEAD_DIM
B_ROT_DIM = B_HEAD_DIM // 4
ROPE_THETA = 500000.0
Q_BLOCK = 128
B_IN_COLS = 3 * D_MODEL

N_EXPERTS = 16
N_GROUPS = 4
EXPERTS_PER_GROUP = N_EXPERTS // N_GROUPS
TOP_K = 2
D_EXPERT = 512
MOE_BLOCK = 128

EPS = 1e-6

kernel_name = "hybrid_mlstm_diffattn_grouped_moe"


def _rms(x, g):
    xf = x.astype(jnp.float32)
    y = xf * lax.rsqrt(jnp.mean(xf * xf, axis=-1, keepdims=True) + EPS)
    return y * g.astype(jnp.float32)


def _chunks(t):
    b_, h_, s_ = t.shape[:3]
    t = t.reshape(b_, h_, s_ // A_CHUNK, A_CHUNK, *t.shape[3:])
    return jnp.moveaxis(t, 2, 0)


def _mlstm_chunkwise(q, k, v, i_pre, log_f):
    b_, h_, s_, dk = q.shape
    dv = v.shape[-1]
    causal = jnp.tril(jnp.ones((A_CHUNK, A_CHUNK), dtype=bool))

    def step(carry, inp):
        C, n, m = carry
        qc, kc, vc, ic, fc = inp
        b = jnp.cumsum(fc, axis=-1)
        log_d = b[..., :, None] - b[..., None, :] + ic[..., None, :]
        log_d = jnp.where(causal, log_d, -jnp.inf)
        log_inter = b + m[..., None]
        m_t = jnp.maximum(jnp.max(log_d, axis=-1), log_inter)
        d = jnp.exp(log_d - m_t[..., None])
        inter = jnp.exp(log_inter - m_t)
        s = jnp.einsum('bhtd,bhsd->bhts', qc, kc) * d
        num = jnp.einsum('bhts,bhsv->bhtv', s, vc) + inter[..., None] * jnp.einsum('bhtd,bhdv->bhtv', qc, C)
        den = jnp.sum(s, axis=-1) + inter * jnp.einsum('bhtd,bhd->bht', qc, n)
        h = num / jnp.maximum(jnp.abs(den), jnp.exp(-m_t))[..., None]
        b_last = b[..., -1]
        log_w = b_last[..., None] - b + ic
        m_new = jnp.maximum(b_last + m, jnp.max(log_w, axis=-1))
        w = jnp.exp(log_w - m_new[..., None])
        decay = jnp.exp(b_last + m - m_new)
        kw = kc * w[..., None]
        C_new = decay[..., None, None] * C + jnp.einsum('bhsd,bhsv->bhdv', kw, vc)
        n_new = decay[..., None] * n + jnp.sum(kw, axis=2)
        return (C_new, n_new, m_new), h

    init = (jnp.zeros((b_, h_, dk, dv), jnp.float32), jnp.zeros((b_, h_, dk), jnp.float32),
            jnp.zeros((b_, h_), jnp.float32))
    _, hs = lax.scan(step, init, (_chunks(q), _chunks(k), _chunks(v), _chunks(i_pre), _chunks(log_f)))
    return jnp.moveaxis(hs, 0, 2).reshape(b_, h_, s_, dv)


def _mlstm_mixer(h, w_in, b_if, h_norm, w_out):
    b_, s_, _ = h.shape
    proj = h @ w_in.astype(h.dtype)
    q = proj[..., :A_HQ].reshape(b_, s_, A_HEADS, A_QK_DIM)
    k = proj[..., A_HQ:2 * A_HQ].reshape(b_, s_, A_HEADS, A_QK_DIM)
    v = proj[..., 2 * A_HQ:2 * A_HQ + A_HV].reshape(b_, s_, A_HEADS, A_V_DIM)
    o = proj[..., 2 * A_HQ + A_HV:2 * A_HQ + 2 * A_HV]
    gates = proj[..., 2 * A_HQ + 2 * A_HV:].astype(jnp.float32) + b_if.astype(jnp.float32)
    i_pre = jnp.transpose(gates[..., :A_HEADS], (0, 2, 1))
    log_f = jax.nn.log_sigmoid(jnp.transpose(gates[..., A_HEADS:], (0, 2, 1)))
    q = jnp.transpose(q, (0, 2, 1, 3)).astype(jnp.float32)
    k = jnp.transpose(k, (0, 2, 1, 3)).astype(jnp.float32) * (A_QK_DIM ** -0.5)
    v = jnp.transpose(v, (0, 2, 1, 3)).astype(jnp.float32)
    hout = _mlstm_chunkwise(q, k, v, i_pre, log_f)
    hout = _rms(jnp.transpose(hout, (0, 2, 1, 3)), h_norm.reshape(A_HEADS, A_V_DIM))
    hout = hout.reshape(b_, s_, A_HV) * jax.nn.sigmoid(o.astype(jnp.float32))
    return hout.astype(h.dtype) @ w_out.astype(h.dtype)


def _partial_rope(t, positions):
    half = B_ROT_DIM // 2
    inv_freq = ROPE_THETA ** (-jnp.arange(0, B_ROT_DIM, 2, dtype=jnp.float32) / B_ROT_DIM)
    ang = positions.astype(jnp.float32)[..., None] * inv_freq
    cos = jnp.cos(ang)[:, :, None, None, :]
    sin = jnp.sin(ang)[:, :, None, None, :]
    x1 = t[..., :half]
    x2 = t[..., half:B_ROT_DIM]
    return jnp.concatenate([x1 * cos - x2 * sin, x2 * cos + x1 * sin, t[..., B_ROT_DIM:]], axis=-1)


def _diff_attention(h, positions, w_in, q_norm, k_norm, lam_q1, lam_k1, lam_q2, lam_k2, o_norm, w_out, layer_idx):
    b_, s_, _ = h.shape
    proj = h @ w_in.astype(h.dtype)
    q = proj[..., :D_MODEL].reshape(b_, s_, B_HEADS, 2, B_HEAD_DIM)
    k = proj[..., D_MODEL:2 * D_MODEL].reshape(b_, s_, B_HEADS, 2, B_HEAD_DIM)
    v = proj[..., 2 * D_MODEL:].reshape(b_, s_, B_HEADS, B_V_DIM).astype(jnp.float32)
    q = _partial_rope(_rms(q, q_norm), positions)
    k = _partial_rope(_rms(k, k_norm), positions)
    q = jnp.transpose(q, (0, 2, 3, 1, 4)) * (B_HEAD_DIM ** -0.5)
    k = jnp.transpose(k, (0, 2, 3, 1, 4))
    v = jnp.transpose(v, (0, 2, 1, 3))
    lam_init = 0.8 - 0.6 * math.exp(-0.3 * layer_idx)
    lam = (jnp.exp(jnp.sum(lam_q1.astype(jnp.float32) * lam_k1.astype(jnp.float32)))
           - jnp.exp(jnp.sum(lam_q2.astype(jnp.float32) * lam_k2.astype(jnp.float32))) + lam_init)
    outs = []
    for j in range(s_ // Q_BLOCK):
        s0 = j * Q_BLOCK
        e = s0 + Q_BLOCK
        sc = jnp.einsum('bhcqd,bhckd->bhcqk', q[:, :, :, s0:e], k[:, :, :, :e])
        mask = (s0 + jnp.arange(Q_BLOCK))[:, None] >= jnp.arange(e)[None, :]
        p = jax.nn.softmax(jnp.where(mask, sc, -jnp.inf), axis=-1)
        a = p[:, :, 0] - lam * p[:, :, 1]
        outs.append(jnp.einsum('bhqk,bhkv->bhqv', a, v[:, :, :e]))
    o = jnp.concatenate(outs, axis=2)
    o = _rms(o, o_norm) * (1.0 - lam_init)
    o = jnp.transpose(o, (0, 2, 1, 3)).reshape(b_, s_, D_MODEL)
    return o.astype(h.dtype) @ w_out.astype(h.dtype)


def _grouped_moe(h, w_router, router_bias, w_gu, w_down):
    b_, s_, d_ = h.shape
    n_tok = b_ * s_
    xt = h.reshape(n_tok, d_)
    scores = jax.nn.sigmoid((xt @ w_router.astype(h.dtype)).astype(jnp.float32))
    biased = (scores + router_bias.astype(jnp.float32)).reshape(n_tok, N_GROUPS, EXPERTS_PER_GROUP)
    grp_score = jnp.sum(lax.top_k(biased, 2)[0], axis=-1)
    g_sel = jnp.argmax(grp_score, axis=-1)
    in_grp = jnp.take_along_axis(biased, g_sel[:, None, None], axis=1)[:, 0]
    _, local_idx = lax.top_k(in_grp, TOP_K)
    expert_idx = g_sel[:, None].astype(jnp.int32) * EXPERTS_PER_GROUP + local_idx.astype(jnp.int32)
    gate_w = jnp.take_along_axis(scores, expert_idx, axis=1)
    gate_w = gate_w / jnp.sum(gate_w, axis=-1, keepdims=True)
    n_assign = n_tok * TOP_K
    n_blocks = (n_assign + N_EXPERTS * (MOE_BLOCK - 1) + MOE_BLOCK - 1) // MOE_BLOCK
    cap = n_blocks * MOE_BLOCK
    flat_e = expert_idx.reshape(n_assign)
    flat_tok = jnp.repeat(jnp.arange(n_tok, dtype=jnp.int32), TOP_K)
    order = jnp.argsort(flat_e, stable=True)
    sorted_e = flat_e[order]
    sorted_tok = flat_tok[order]
    sorted_w = gate_w.reshape(n_assign)[order]
    counts = jnp.bincount(flat_e, length=N_EXPERTS).astype(jnp.int32)
    starts = jnp.cumsum(counts) - counts
    padded = ((counts + MOE_BLOCK - 1) // MOE_BLOCK) * MOE_BLOCK
    pad_ends = jnp.cumsum(padded)
    pad_starts = pad_ends - padded
    dest = pad_starts[sorted_e] + (jnp.arange(n_assign, dtype=jnp.int32) - starts[sorted_e])
    buf_tok = jnp.full((cap,), n_tok, jnp.int32).at[dest].set(sorted_tok)
    x_pad = jnp.concatenate([xt, jnp.zeros((1, d_), xt.dtype)], axis=0)
    xb = x_pad[buf_tok].reshape(n_blocks, MOE_BLOCK, d_)
    block_e = jnp.minimum(jnp.searchsorted(pad_ends, jnp.arange(n_blocks, dtype=jnp.int32) * MOE_BLOCK, side='right'),
                          N_EXPERTS - 1)

    def expert_block(args):
        xblk, e = args
        gu = xblk @ w_gu[e].astype(xblk.dtype)
        act = jax.nn.silu(gu[:, :D_EXPERT]) * gu[:, D_EXPERT:]
        return act @ w_down[e].astype(xblk.dtype)

    yb = lax.map(expert_block, (xb, block_e)).reshape(cap, d_)
    contrib = yb[dest].astype(jnp.float32) * sorted_w[:, None]
    y = jax.ops.segment_sum(contrib, sorted_tok, num_segments=n_tok)
    return y.reshape(b_, s_, d_).astype(h.dtype)


def setup_inputs(seed: int = 0) -> dict:
    key = jax.random.key(seed)
    ks = jax.random.split(key, 24)
    f32 = jnp.float32
    nrm = lambda k, shape, s: jax.random.normal(k, shape, f32) * s
    offs = jax.random.randint(ks[2], (BATCH, 1), 0, 4096, dtype=jnp.int32)
    positions = offs + jnp.arange(SEQ, dtype=jnp.int32)[None, :]
    f_bias = jnp.linspace(3.0, 6.0, A_HEADS, dtype=f32)[None, :] + nrm(ks[8], (N_LAYERS_A, A_HEADS), 0.1)
    i_bias = nrm(ks[9], (N_LAYERS_A, A_HEADS), 0.1)
    return {
        "x": nrm(ks[0], (BATCH, SEQ, D_MODEL), 1.0),
        "c": nrm(ks[1], (BATCH, D_MODEL), 1.0),
        "positions": positions,
        "norm1": 1.0 + nrm(ks[3], (DEPTH, D_MODEL), 0.02),
        "norm2": 1.0 + nrm(ks[4], (DEPTH, D_MODEL), 0.02),
        "w_ada": nrm(ks[5], (DEPTH, D_MODEL, 6 * D_MODEL), 0.5 * D_MODEL ** -0.5),
        "b_ada": nrm(ks[6], (DEPTH, 6 * D_MODEL), 0.02),
        "a_w_in": nrm(ks[7], (N_LAYERS_A, D_MODEL, A_IN_COLS), D_MODEL ** -0.5),
        "a_b_if": jnp.concatenate([i_bias, f_bias], axis=-1),
        "a_h_norm": 1.0 + nrm(ks[10], (N_LAYERS_A, A_HV), 0.02),
        "a_w_out": nrm(ks[11], (N_LAYERS_A, A_HV, D_MODEL), A_HV ** -0.5),
        "b_w_in": nrm(ks[12], (N_LAYERS_B, D_MODEL, B_IN_COLS), D_MODEL ** -0.5),
        "b_q_norm": 1.0 + nrm(ks[13], (N_LAYERS_B, B_HEAD_DIM), 0.02),
        "b_k_norm": 1.0 + nrm(ks[14], (N_LAYERS_B, B_HEAD_DIM), 0.02),
        "b_lam_q1": nrm(ks[15], (N_LAYERS_B, B_HEAD_DIM), 0.1),
        "b_lam_k1": nrm(ks[16], (N_LAYERS_B, B_HEAD_DIM), 0.1),
        "b_lam_q2": nrm(ks[17], (N_LAYERS_B, B_HEAD_DIM), 0.1),
        "b_lam_k2": nrm(ks[18], (N_LAYERS_B, B_HEAD_DIM), 0.1),
        "b_o_norm": 1.0 + nrm(ks[19], (N_LAYERS_B, B_V_DIM), 0.02),
        "b_w_out": nrm(ks[20], (N_LAYERS_B, D_MODEL, D_MODEL), D_MODEL ** -0.5),
        "w_router": nrm(ks[21], (D_MODEL, N_EXPERTS), D_MODEL ** -0.5),
        "router_bias": nrm(ks[22], (N_EXPERTS,), 0.01),
        "moe_w_gu": nrm(ks[23], (DEPTH, N_EXPERTS, D_MODEL, 2 * D_EXPERT), D_MODEL ** -0.5),
        "moe_w_down": nrm(jax.random.fold_in(key, 99), (DEPTH, N_EXPERTS, D_EXPERT, D_MODEL), D_EXPERT ** -0.5),
    }


def reference(x, c, positions, norm1, norm2, w_ada, b_ada, a_w_in, a_b_if, a_h_norm, a_w_out,
              b_w_in, b_q_norm, b_k_norm, b_lam_q1, b_lam_k1, b_lam_q2, b_lam_k2, b_o_norm, b_w_out,
              w_router, router_bias, moe_w_gu, moe_w_down):
    c_act = jax.nn.silu(c.astype(jnp.float32))
    for l in range(DEPTH):
        mod = (c_act @ w_ada[l].astype(jnp.float32) + b_ada[l].astype(jnp.float32))[:, None, :]
        sh1, sc1, g1, sh2, sc2, g2 = jnp.split(mod, 6, axis=-1)
        h = (_rms(x, norm1[l]) * (1.0 + sc1) + sh1).astype(x.dtype)
        j = l // N_MIXERS
        if l % N_MIXERS == 0:
            y = _mlstm_mixer(h, a_w_in[j], a_b_if[j], a_h_norm[j], a_w_out[j])
        else:
            y = _diff_attention(h, positions, b_w_in[j], b_q_norm[j], b_k_norm[j], b_lam_q1[j], b_lam_k1[j],
                                b_lam_q2[j], b_lam_k2[j], b_o_norm[j], b_w_out[j], l)
        x = (x.astype(jnp.float32) + g1 * y.astype(jnp.float32)).astype(x.dtype)
        h = (_rms(x, norm2[l]) * (1.0 + sc2) + sh2).astype(x.dtype)
        y = _grouped_moe(h, w_router, router_bias, moe_w_gu[l], moe_w_down[l])
        x = (x.astype(jnp.float32) + g2 * y.astype(jnp.float32)).astype(x.dtype)
    return x
```

```python
import math
from contextlib import ExitStack

import numpy as np
import concourse.bass as bass
import concourse.mybir as mybir
from concourse.bass_utils import run_bass_kernel_spmd

F32 = mybir.dt.float32
BF16 = mybir.dt.bfloat16
I32 = mybir.dt.int32
AF = mybir.ActivationFunctionType
ALU = mybir.AluOpType
AX = mybir.AxisListType

D = 1024
KC = 8
N_CORES = 8
SEQ = 2048
BATCH = 16
DEPTH = 2
NE = 16
DE = 512
EPS = 1e-6
A_HEADS = 4
B_HEADS = 8
ROPE_THETA = 500000.0


class Res:
    __slots__ = ("name", "w", "r", "dsem")

    def __init__(self, name):
        self.name = name
        self.w = {}
        self.r = {}
        self.dsem = None


class Eng:
    def __init__(self, name, h, semi):
        self.name = name
        self.h = h
        self.semi = semi
        self.n = 0
        self.seen = {}


class K:
    def __init__(self, nc, stack):
        self.nc = nc
        self.stack = stack
        self.semh = []
        self.dma_tot = {}
        self.engs = {}
        for name, h in (("pe", nc.tensor), ("act", nc.scalar), ("dve", nc.vector),
                        ("pool", nc.gpsimd), ("sp", nc.sync)):
            self.engs[name] = Eng(name, h, self._newsem("e_" + name))
        self.n_ins = 0

    def _newsem(self, name):
        s = self.stack.enter_context(self.nc.semaphore("%s_%d" % (name, len(self.semh))))
        self.semh.append(s)
        return len(self.semh) - 1

    def res(self, name):
        return Res(name)

    def _gather(self, e, reads, writes):
        need = {}
        own = e.semi
        for r in reads:
            for s, v in r.w.items():
                if s == own and e.name == "pe":
                    continue
                if need.get(s, 0) < v:
                    need[s] = v
        for w in writes:
            for s, v in w.w.items():
                if s == own and e.name == "pe":
                    continue
                if need.get(s, 0) < v:
                    need[s] = v
            for s, v in w.r.items():
                if s == own and e.name == "pe":
                    continue
                if need.get(s, 0) < v:
                    need[s] = v
        for s in list(need):
            if s in self.dma_tot:
                need[s] = self.dma_tot[s]
        for s, v in need.items():
            if e.seen.get(s, 0) < v:
                e.h.wait_ge(self.semh[s], v)
                e.seen[s] = v

    def _record(self, ev, reads, writes):
        s, v = ev
        for w in writes:
            w.w = {s: v}
            w.r = {}
        for r in reads:
            if r.r.get(s, 0) < v:
                r.r[s] = v

    def op(self, en, fn, reads=(), writes=()):
        e = self.engs[en]
        self._gather(e, reads, writes)
        ins = fn(e.h)
        e.n += 1
        ins.then_inc(self.semh[e.semi], 1)
        self._record((e.semi, e.n), reads, writes)
        self.n_ins += 1
        return ins

    def dma(self, qn, out, in_, reads=(), writes=(), sem_res=None, **kw):
        e = self.engs[qn]
        self._gather(e, reads, writes)
        if sem_res.dsem is None:
            sem_res.dsem = {}
        kind = "sw" if qn == "pool" else "hw"
        if kind not in sem_res.dsem:
            sem_res.dsem[kind] = self._newsem("d%s_%s" % (kind, sem_res.name))
            self.dma_tot[sem_res.dsem[kind]] = 0
        s = sem_res.dsem[kind]
        ins = e.h.dma_start(out=out, in_=in_, **kw)
        self.dma_tot[s] += 16
        ins.then_inc(self.semh[s], 16)
        self._record((s, self.dma_tot[s]), reads, writes)
        self.n_ins += 1
        return ins

    def wait_res(self, en, res_list):
        self._gather(self.engs[en], res_list, [])

    def barrier(self):
        tgt = {e.semi: e.n for e in self.engs.values() if e.n > 0}
        for s, v in self.dma_tot.items():
            if v > 0:
                tgt[s] = v
        for e in self.engs.values():
            for s, v in tgt.items():
                if s == e.semi and e.name == "pe":
                    continue
                if e.seen.get(s, 0) < v:
                    e.h.wait_ge(self.semh[s], v)
                    e.seen[s] = v

    def final_wait(self, res_list):
        e = self.engs["sp"]
        need = {}
        for r in res_list:
            for s, v in list(r.w.items()) + list(r.r.items()):
                need[s] = max(need.get(s, 0), v)
        for s in list(need):
            if s in self.dma_tot:
                need[s] = self.dma_tot[s]
        for s, v in need.items():
            e.h.wait_ge(self.semh[s], v)


class Prog:
    def __init__(self, S=SEQ, NB=2, debug=None, layers=(0, 1), do_mixer=True, do_moe=True, stage=99):
        self.stage = stage
        self.S = S
        self.NB = NB
        self.NT = S // 128
        self.NG = max(1, S // 512)
        self.GW = min(512, S)
        self.debug = debug
        self.layers = layers
        self.do_mixer = do_mixer
        self.do_moe = do_moe
        self.nc = bass.Bass("TRN2", target_bir_lowering=False)
        self.stack = ExitStack()
        self._uid = 0

    def sb(self, shape, dt, name=None, stack=None):
        if getattr(self, "trace_alloc", False):
            print("ALLOC", name, shape, dt, "remaining", self.nc.sbuf_bytes_remaining)
        self._uid += 1
        nm = "%s_%d" % (name or "t", self._uid)
        st = stack if stack is not None else self.stack
        return st.enter_context(self.nc.sbuf_tensor(nm, list(shape), dt))

    def dram_in(self, name, shape, dt=F32):
        return self.nc.dram_tensor(name, list(shape), dt, kind="ExternalInput").ap()

    def run_interleaved(self, work, W, body):
        work = list(work)
        free = list(range(W))
        active = []
        wi = 0
        while wi < len(work) or active:
            while free and wi < len(work):
                slot = free.pop(0)
                active.append((slot, body(*work[wi], slot)))
                wi += 1
            nxt = []
            for slot, g_ in active:
                try:
                    next(g_)
                    nxt.append((slot, g_))
                except StopIteration:
                    free.append(slot)
            active = nxt

    def psum(self):
        i = self.ps_i
        self.ps_i = (i + 1) % len(self.ps_tiles)
        return self.ps_tiles[i], self.ps_res[i]

    def build(self):
        nc, S, NB, NT = self.nc, self.S, self.NB, self.NT
        st = self.stack
        k = self.k = K(nc, st)
        self.x_d = self.dram_in("x", [NB, S, D])
        self.out_d = nc.dram_tensor("out", [NB, S, D], F32, kind="ExternalOutput").ap()
        self.cT_d = self.dram_in("cT", [128, KC * NB])
        self.vecT_d = self.dram_in("vecT", [128, self.NV])
        self.pos_d = self.dram_in("posT", [128, NB * NT], I32)
        self.w_ada_d = self.dram_in("w_ada", [DEPTH, D, 6 * D])
        self.a_w_in_d = self.dram_in("a_w_in", [D, 3080])
        self.a_w_out_d = self.dram_in("a_w_out", [D, D])
        self.b_w_in_d = self.dram_in("b_w_in", [D, 3 * D])
        self.b_w_out_d = self.dram_in("b_w_out", [D, D])
        self.w_router_d = self.dram_in("w_router", [D, NE])
        self.rbias_d = self.dram_in("rbias", [1, NE])
        self.small_d = self.dram_in("small", [1, self.NSMALL])
        self.w_gu_d = self.dram_in("moe_w_gu", [DEPTH, NE, D, 2 * DE])
        self.w_dn_d = self.dram_in("moe_w_down", [DEPTH, NE, DE, D])
        if self.debug:
            self.dbg_d = {nm: nc.dram_tensor("dbg_" + nm, list(shp), F32, kind="ExternalOutput").ap()
                          for nm, shp in self.debug.items()}

        self.x_sb = self.sb([128, NT, D], F32, "x")
        self.xr = [k.res("x%d" % i) for i in range(NT)]
        self.hT = self.sb([128, KC, S], BF16, "hT")
        self.hTr = [k.res("hT%d" % g) for g in range(self.NG)]
        self.ps_tiles = [st.enter_context(nc.psum_tensor("ps%d" % i, [128, 512], F32)) for i in range(8)]
        self.ps_res = [k.res("ps%d" % i) for i in range(8)]
        self.ps_i = 0

        self.setup_consts()
        self.adaln()
        for b in range(NB):
            self.sequence(b)
        k.final_wait(self.final_res)
        return nc

    NV = 2 * 64 + 8
    NSMALL = 8 + 64 * 6 + 128

    def setup_consts(self):
        k, nc = self.k, self.nc
        self.c_res = k.res("consts")
        cr = self.c_res
        self.ident_f = self.sb([128, 128], F32, "identf")
        self.ident_b = self.sb([128, 128], BF16, "identb")
        self.maskadd = self.sb([128, 128], F32, "maskadd")
        self.mask01 = self.sb([128, 128], BF16, "mask01")
        self.ones_f = self.sb([128, 128], F32, "onesf")
        self.iot = self.sb([128, 128], F32, "iot")
        k.op("pool", lambda h: h.iota(self.iot[:], pattern=[[1, 128]], base=0, channel_multiplier=-1,
                                      allow_small_or_imprecise_dtypes=True), writes=[cr])
        k.op("dve", lambda h: h.tensor_single_scalar(self.ident_f[:], self.iot[:], 0.0, op=ALU.is_equal),
             reads=[cr], writes=[cr])
        k.op("dve", lambda h: h.tensor_copy(self.ident_b[:], self.ident_f[:]), reads=[cr], writes=[cr])
        k.op("dve", lambda h: h.tensor_single_scalar(self.mask01[:], self.iot[:], 0.0, op=ALU.is_ge),
             reads=[cr], writes=[cr])
        k.op("dve", lambda h: h.tensor_scalar(self.maskadd[:], self.iot[:], 0.0, 30000.0,
                                              op0=ALU.is_ge, op1=ALU.mult), reads=[cr], writes=[cr])
        k.op("dve", lambda h: h.tensor_scalar_add(self.maskadd[:], self.maskadd[:], -30000.0),
             reads=[cr], writes=[cr])
        k.op("dve", lambda h: h.memset(self.ones_f[:], 1.0), writes=[cr])
        self.vecT = self.sb([128, self.NV], F32, "vecT")
        self.cT = self.sb([128, KC * self.NB], F32, "cT")
        self.posT = self.sb([128, self.NB * self.NT], I32, "posT")
        self.rbias_b = self.sb([128, NE], F32, "rbias")
        self.small_b = self.sb([128, self.NSMALL], F32, "smallb")
        self.wr_sb = self.sb([128, KC, NE], BF16, "wrouter")
        k.dma("sp", self.vecT[:], self.vecT_d[:, :], writes=[cr], sem_res=cr)
        k.dma("sp", self.cT[:], self.cT_d[:, :], writes=[cr], sem_res=cr)
        k.dma("sp", self.posT[:], self.pos_d[:, :], writes=[cr], sem_res=cr)
        k.dma("sp", self.rbias_b[:], self.rbias_d.partition_broadcast(128), writes=[cr], sem_res=cr)
        k.dma("sp", self.small_b[:], self.small_d.partition_broadcast(128), writes=[cr], sem_res=cr)
        k.dma("pool", self.wr_sb[:], self.w_router_d.rearrange("(kc p) e -> p kc e", p=128),
              writes=[cr], sem_res=cr)
        self.onorm_col = self.sb([128, 1], F32, "onormc")
        k.dma("sp", self.onorm_col[:], self.small_d[0:1, 392:520].rearrange("o (p u) -> (o p) u", u=1),
              writes=[cr], sem_res=cr, allow_slow_non_contiguous=True)
        self.cact = self.sb([128, KC * self.NB], F32, "cact")
        k.op("act", lambda h: h.activation(out=self.cact[:], in_=self.cT[:], func=AF.Silu),
             reads=[cr], writes=[cr])

    def adaln(self):
        k, nc, NB = self.k, self.nc, self.NB
        self.modT = self.sb([128, DEPTH * 48 * NB], F32, "modT")
        self.mod_res = k.res("modT")
        CB = 512
        nblk = 6 * D // CB
        ada_stack = ExitStack()
        wbuf = [self.sb([128, KC, CB], F32, "wada%d" % i, ada_stack) for i in range(2)]
        wres = [k.res("wada%d" % i) for i in range(2)]
        it = 0
        for l in range(DEPTH):
            if l not in self.layers:
                continue
            for blk in range(nblk):
                wb, wr = wbuf[it % 2], wres[it % 2]
                it += 1
                k.dma("sp", wb[:], self.w_ada_d[l, :, blk * CB:(blk + 1) * CB].rearrange("(kc p) n -> p kc n", p=128),
                      writes=[wr], sem_res=wr)
                ps, pr = self.psum()
                for jj in range(CB // 128):
                    j = blk * (CB // 128) + jj
                    for kc in range(KC):
                        k.op("pe", lambda h, jj=jj, kc=kc: h.matmul(
                            ps[:, jj * NB:(jj + 1) * NB], lhsT=wb[:, kc, jj * 128:(jj + 1) * 128],
                            rhs=self.cact[:, kc * NB:(kc + 1) * NB], start=(kc == 0), stop=(kc == KC - 1)),
                            reads=[wr, self.c_res], writes=[pr])
                    col = (l * 48 + j) * NB
                    bcol = l * 64 + j
                    k.op("dve", lambda h, jj=jj, col=col, bcol=bcol: h.tensor_scalar(
                        self.modT[:, col:col + NB], ps[:, jj * NB:(jj + 1) * NB],
                        self.vecT[:, bcol:bcol + 1], None, op0=ALU.add),
                        reads=[pr, self.c_res], writes=[self.mod_res])
        k.barrier()
        ada_stack.close()

    def mod_col(self, l, which, b):
        NB = self.NB
        base = (l * 48 + which * 8) * NB + b
        return self.modT[:, base:base + 7 * NB + 1:NB]

    def sequence(self, b):
        k, nc, NT = self.k, self.nc, self.NT
        for i in range(NT):
            k.dma("sp", self.x_sb[:, i, :], self.x_d[b, i * 128:(i + 1) * 128, :],
                  writes=[self.xr[i]], sem_res=self.xr[0])
        for l in self.layers:
            if self.do_mixer:
                self.norm_to_hT(b, l, 0)
                k.barrier()
                if l % 2 == 0:
                    self.mlstm(b, l)
                else:
                    self.diffattn(b, l)
                k.barrier()
            if self.do_moe:
                self.norm_to_hT(b, l, 1)
                k.barrier()
                self.moe(b, l)
                k.barrier()
        outr = self.k.res("out%d" % b)
        for i in range(NT):
            k.dma("sp", self.out_d[b, i * 128:(i + 1) * 128, :], self.x_sb[:, i, :],
                  reads=[self.xr[i]], writes=[outr], sem_res=outr)
        self.final_res = getattr(self, "final_res", []) + [outr]

    def norm_to_hT(self, b, l, which):
        k, nc, NT, NB = self.k, self.nc, self.NT, self.NB
        with ExitStack() as ls:
            ssq = self.sb([128, NT], F32, "ssq", ls)
            rstd = self.sb([128, NT], F32, "rstd", ls)
            junk = self.sb([128, D], BF16, "junk", ls)
            W = self.sb([128, 8], F32, "W", ls)
            xn = [self.sb([128, D], BF16, "xn", ls) for _ in range(2)]
            r_s = k.res("ssq")
            r_j = k.res("junk")
            r_W = k.res("W")
            r_xn = [k.res("xn0"), k.res("xn1")]
            ncol = l * 64 + 48 + which * 8
            sc = self.mod_col(l, which * 3 + 1, b)
            sh = self.mod_col(l, which * 3 + 0, b)
            k.op("dve", lambda h: h.scalar_tensor_tensor(W[:], sc, 1.0, self.vecT[:, ncol:ncol + 8],
                                                         op0=ALU.add, op1=ALU.mult),
                 reads=[self.mod_res, self.c_res], writes=[r_W])
            for i in range(NT):
                k.op("act", lambda h, i=i: h.activation(out=junk[:], in_=self.x_sb[:, i, :], func=AF.Square,
                                                        accum_out=ssq[:, i:i + 1]),
                     reads=[self.xr[i]], writes=[r_j, r_s])
            k.op("dve", lambda h: h.tensor_scalar(rstd[:], ssq[:], 1.0 / D, EPS, op0=ALU.mult, op1=ALU.add),
                 reads=[r_s], writes=[r_s])
            k.op("act", lambda h: h.activation(out=rstd[:], in_=rstd[:], func=AF.Sqrt), reads=[r_s], writes=[r_s])
            k.op("dve", lambda h: h.reciprocal(rstd[:], rstd[:]), reads=[r_s], writes=[r_s])
            def emit_xn(i):
                xb, xr_ = xn[i % 2], r_xn[i % 2]
                k.op("act", lambda h, i=i, xb=xb: h.activation(out=xb[:], in_=self.x_sb[:, i, :], func=AF.Copy,
                                                               scale=rstd[:, i:i + 1]),
                     reads=[self.xr[i], r_s], writes=[xr_])

            emit_xn(0)
            for i in range(NT):
                xb, xr_ = xn[i % 2], r_xn[i % 2]
                ps, pr = self.psum()
                psb = ps[:].bitcast(BF16)
                for j in range(KC):
                    k.op("pe", lambda h, j=j, xb=xb: h.transpose(psb[:, j * 128:(j + 1) * 128],
                                                                 xb[:, j * 128:(j + 1) * 128], self.ident_b[:]),
                         reads=[xr_, self.c_res], writes=[pr])
                if i + 1 < NT:
                    emit_xn(i + 1)
                g = (i * 128) // self.GW
                for j in range(KC):
                    en = "dve" if j % 2 == 0 else "act"
                    dst = self.hT[:, j, i * 128:(i + 1) * 128]
                    src = psb[:, j * 128:(j + 1) * 128]
                    if en == "dve":
                        k.op("dve", lambda h, dst=dst, src=src, j=j: h.tensor_scalar(
                            dst, src, W[:, j:j + 1], sh[:, j:j + 1], op0=ALU.mult, op1=ALU.add),
                            reads=[pr, r_W, self.mod_res], writes=[self.hTr[g]])
                    else:
                        k.op("act", lambda h, dst=dst, src=src, j=j: h.activation(
                            out=dst, in_=src, func=AF.Identity, scale=W[:, j:j + 1], bias=sh[:, j:j + 1]),
                            reads=[pr, r_W, self.mod_res], writes=[self.hTr[g]])

    def bcast_row(self, col8, dst, dst_res, reads):
        k = self.k
        with ExitStack() as ls:
            dg = [self.sb([128, 128], F32, "diag", ls) for _ in range(2)]
            dr = [k.res("diag0"), k.res("diag1")]
            for j in range(KC):
                d_, r_ = dg[j % 2], dr[j % 2]
                k.op("dve", lambda h, d_=d_, j=j: h.tensor_scalar(d_[:], self.ident_f[:], col8[:, j:j + 1], None,
                                                                  op0=ALU.mult),
                     reads=list(reads) + [self.c_res], writes=[r_])
                ps, pr = self.psum()
                k.op("pe", lambda h, d_=d_: h.matmul(ps[:, 0:128], lhsT=self.ones_f[:], rhs=d_[:],
                                                     start=True, stop=True),
                     reads=[r_, self.c_res], writes=[pr])
                k.op("act", lambda h, j=j: h.activation(out=dst[:, j * 128:(j + 1) * 128], in_=ps[:, 0:128],
                                                        func=AF.Copy),
                     reads=[pr], writes=[dst_res])
            k.barrier()

    def moe(self, b, l):
        k, nc, NT, NG, GW = self.k, self.nc, self.NT, self.NG, self.GW
        TPG = GW // 128
        with ExitStack() as ls:
            G2b = self.sb([128, D], F32, "G2b", ls)
            r_G2 = k.res("G2b")
            self.bcast_row(self.mod_col(l, 5, b), G2b, r_G2, [self.mod_res])
            wgu = [self.sb([128, KC, 2 * DE], BF16, "wgu", ls) for _ in range(2)]
            wdn = [self.sb([128, DE // 128, D], BF16, "wdn", ls) for _ in range(2)]
            r_w = [k.res("wexp0"), k.res("wexp1")]
            def load_w(e):
                bi = e % 2
                k.dma("pool", wgu[bi][:], self.w_gu_d[l, e].rearrange("(kc p) n -> p kc n", p=128),
                      writes=[r_w[bi]], sem_res=r_w[bi])
                k.dma("pool", wdn[bi][:], self.w_dn_d[l, e].rearrange("(kc p) n -> p kc n", p=128),
                      writes=[r_w[bi]], sem_res=r_w[bi])

            load_w(0)
            gates = self.sb([128, NT, NE], F32, "gates", ls)
            r_gates = k.res("gates")
            RT = NT * NE
            r_rt = k.res("router_tmp")
            rt = {nm: self.sb([128, NT, NE], F32, nm, ls) for nm in ("scores", "biased", "b2", "ge", "gs")}
            rs = {nm: self.sb([128, NT, 4], F32, nm, ls) for nm in ("m1", "m2", "gsc", "gsel")}
            r1 = {nm: self.sb([128, NT], F32, nm, ls) for nm in ("gmax", "den")}

            def fl(t):
                return t[:].rearrange("p i e -> p (i e)")

            def v4(t):
                return t[:].rearrange("p i (g e) -> p i g e", g=4)

            def b4(t):
                return t[:].unsqueeze(3).broadcast_to([128, NT, 4, 4])

            ps, pr = self.psum()
            for i in range(NT):
                g = (i * 128) // GW
                for kc in range(KC):
                    k.op("pe", lambda h, kc=kc, i=i: h.matmul(ps[:, i * NE:(i + 1) * NE], lhsT=self.hT[:, kc, i * 128:(i + 1) * 128],
                                                          rhs=self.wr_sb[:, kc, :], start=(kc == 0), stop=(kc == KC - 1)),
                         reads=[self.hTr[g], self.c_res], writes=[pr])
            k.op("act", lambda h: h.activation(out=fl(rt["scores"]), in_=ps[:, 0:RT], func=AF.Sigmoid),
                 reads=[pr], writes=[r_rt])
            dv = lambda fn: k.op("dve", fn, reads=[r_rt, self.c_res], writes=[r_rt])
            dv(lambda h: h.tensor_tensor(rt["biased"][:], rt["scores"][:],
                                         self.rbias_b[:].unsqueeze(1).broadcast_to([128, NT, NE]), op=ALU.add))
            dv(lambda h: h.tensor_reduce(rs["m1"][:], v4(rt["biased"]), axis=AX.X, op=ALU.max))
            dv(lambda h: h.tensor_tensor(v4(rt["ge"]), v4(rt["biased"]), b4(rs["m1"]), op=ALU.is_equal))
            dv(lambda h: h.scalar_tensor_tensor(fl(rt["b2"]), fl(rt["ge"]), -1000.0, fl(rt["biased"]),
                                                op0=ALU.mult, op1=ALU.add))
            dv(lambda h: h.tensor_reduce(rs["m2"][:], v4(rt["b2"]), axis=AX.X, op=ALU.max))
            dv(lambda h: h.tensor_add(rs["gsc"][:], rs["m1"][:], rs["m2"][:]))
            dv(lambda h: h.tensor_reduce(r1["gmax"][:], rs["gsc"][:], axis=AX.X, op=ALU.max))
            dv(lambda h: h.tensor_tensor(rs["gsel"][:], rs["gsc"][:],
                                         r1["gmax"][:].unsqueeze(2).broadcast_to([128, NT, 4]), op=ALU.is_ge))
            dv(lambda h: h.tensor_tensor(v4(rt["ge"]), v4(rt["biased"]), b4(rs["m2"]), op=ALU.is_ge))
            dv(lambda h: h.tensor_tensor(v4(rt["ge"]), v4(rt["ge"]), b4(rs["gsel"]), op=ALU.mult))
            dv(lambda h: h.tensor_mul(fl(rt["gs"]), fl(rt["ge"]), fl(rt["scores"])))
            dv(lambda h: h.tensor_reduce(r1["den"][:], rt["gs"][:], axis=AX.X, op=ALU.add))
            dv(lambda h: h.reciprocal(r1["den"][:], r1["den"][:]))
            k.op("dve", lambda h: h.tensor_tensor(gates[:], rt["gs"][:],
                                                  r1["den"][:].unsqueeze(2).broadcast_to([128, NT, NE]), op=ALU.mult),
                 reads=[r_rt], writes=[r_gates])
            if self.debug and "gates" in self.debug and b == 0:
                dr = k.res("dbg_gates")
                k.dma("sp", self.dbg_d["gates"].rearrange("(i p) e -> p i e", p=128), gates[:],
                      reads=[r_gates], writes=[dr], sem_res=dr)
                self.final_res = getattr(self, "final_res", []) + [dr]
            act = [self.sb([128, DE // 128, GW], BF16, "act", ls) for _ in range(2)]
            r_act = [k.res("act0"), k.res("act1")]
            sg = [self.sb([128, GW], F32, "sg", ls) for _ in range(2)]
            r_sg = [k.res("sg0"), k.res("sg1")]
            tmp = [self.sb([128, 512], F32, "tmp", ls) for _ in range(2)]
            r_tmp = [k.res("tmp0"), k.res("tmp1")]

            cnt = [0]

            def emit_gu(e, g, n_):
                bi = e % 2
                a_, ra_ = act[n_ % 2], r_act[n_ % 2]
                for fc in range(DE // 128):
                    psg, prg = self.psum()
                    psu, pru = self.psum()
                    for kc in range(KC):
                        k.op("pe", lambda h, kc=kc, fc=fc: h.matmul(
                            psg[:, 0:GW], lhsT=wgu[bi][:, kc, fc * 128:(fc + 1) * 128],
                            rhs=self.hT[:, kc, g * GW:(g + 1) * GW], start=(kc == 0), stop=(kc == KC - 1)),
                            reads=[r_w[bi], self.hTr[g]], writes=[prg])
                    for kc in range(KC):
                        k.op("pe", lambda h, kc=kc, fc=fc: h.matmul(
                            psu[:, 0:GW], lhsT=wgu[bi][:, kc, DE + fc * 128:DE + (fc + 1) * 128],
                            rhs=self.hT[:, kc, g * GW:(g + 1) * GW], start=(kc == 0), stop=(kc == KC - 1)),
                            reads=[r_w[bi], self.hTr[g]], writes=[pru])
                    s_, rs_ = sg[cnt[0] % 2], r_sg[cnt[0] % 2]
                    cnt[0] += 1
                    k.op("act", lambda h, s_=s_: h.activation(out=s_[:], in_=psg[:, 0:GW], func=AF.Silu),
                         reads=[prg], writes=[rs_])
                    k.op("dve", lambda h, s_=s_, fc=fc, a_=a_: h.tensor_tensor(a_[:, fc, :], s_[:], psu[:, 0:GW],
                                                                             op=ALU.mult),
                         reads=[rs_, pru], writes=[ra_])

            def emit_down(e, g, n_):
                bi = e % 2
                a_, ra_ = act[n_ % 2], r_act[n_ % 2]
                for tt in range(TPG):
                    i = g * TPG + tt
                    for dh in range(2):
                        pso, pro = self.psum()
                        for fc in range(DE // 128):
                            k.op("pe", lambda h, fc=fc, tt=tt, dh=dh: h.matmul(
                                pso[:], lhsT=a_[:, fc, tt * 128:(tt + 1) * 128],
                                rhs=wdn[bi][:, fc, dh * 512:(dh + 1) * 512],
                                start=(fc == 0), stop=(fc == DE // 128 - 1)),
                                reads=[ra_, r_w[bi]], writes=[pro])
                        t_, rt_ = tmp[cnt[0] % 2], r_tmp[cnt[0] % 2]
                        cnt[0] += 1
                        k.op("dve", lambda h, t_=t_, i=i, dh=dh: h.scalar_tensor_tensor(
                            t_[:], pso[:], gates[:, i, e:e + 1], G2b[:, dh * 512:(dh + 1) * 512],
                            op0=ALU.mult, op1=ALU.mult),
                            reads=[pro, r_gates, r_G2], writes=[rt_])
                        k.op("pool", lambda h, t_=t_, i=i, dh=dh: h.tensor_tensor(
                            self.x_sb[:, i, dh * 512:(dh + 1) * 512], self.x_sb[:, i, dh * 512:(dh + 1) * 512],
                            t_[:], op=ALU.add),
                            reads=[rt_, self.xr[i]], writes=[self.xr[i]])

            steps = [(e, g, e * NG + g) for e in range(NE) for g in range(NG)]
            prev = None
            for (e, g, n_) in steps:
                if g == 0 and e + 1 < NE:
                    if prev is not None:
                        emit_down(*prev)
                        prev = None
                    load_w(e + 1)
                emit_gu(e, g, n_)
                if prev is not None:
                    emit_down(*prev)
                prev = (e, g, n_)
            emit_down(*prev)
            k.barrier()

    def mlstm(self, b, l):
        k, nc, NT, NG, GW, S = self.k, self.nc, self.NT, self.NG, self.GW, self.S
        TPG = GW // 128
        H = A_HEADS
        win = self.a_w_in_d
        with ExitStack() as ls:
            Bg_col = self.sb([128, NT, H], F32, "Bgc", ls)
            G_col = self.sb([128, NT, H], F32, "Gc", ls)
            thr_col = self.sb([128, NT, H], F32, "thrc", ls)
            r_cols = k.res("gcols")
            with ExitStack() as ps_:
                wg = self.sb([128, KC, 8], BF16, "wg", ps_)
                r_wg = k.res("wg")
                k.dma("pool", wg[:], win[:, 3072:3080].rearrange("(kc p) n -> p kc n", p=128),
                      writes=[r_wg], sem_res=r_wg)
                bcol = self.sb([4, 2], F32, "bcol", ps_)
                k.dma("sp", bcol[:], self.small_d[0:1, 0:8].rearrange("o (t h) -> (o h) t", t=2),
                      writes=[r_wg], sem_res=r_wg, allow_slow_non_contiguous=True)
                nbf = self.sb([4, 1], F32, "nbf", ps_)
                k.op("dve", lambda h: h.tensor_scalar(nbf[:], bcol[:, 1:2], -1.0, None, op0=ALU.mult),
                     reads=[r_wg], writes=[r_wg])
                A = self.sb([4, S], F32, "rowA", ps_)
                Bt = self.sb([4, S], F32, "rowB", ps_)
                C = self.sb([4, S], F32, "rowC", ps_)
                ones_r = self.sb([4, S], F32, "rowOnes", ps_)
                r_rows = k.res("rows")
                k.op("dve", lambda h: h.memset(ones_r[:], 1.0), writes=[r_rows])
                for g in range(NG):
                    sl = slice(g * GW, (g + 1) * GW)
                    psi, pri = self.psum()
                    for kc in range(KC):
                        k.op("pe", lambda h, kc=kc: h.matmul(psi[0:4, 0:GW], lhsT=wg[:, kc, 0:4], rhs=self.hT[:, kc, sl],
                                                             start=(kc == 0), stop=(kc == KC - 1)),
                             reads=[r_wg, self.hTr[g]], writes=[pri])
                    psf, prf = self.psum()
                    for kc in range(KC):
                        k.op("pe", lambda h, kc=kc: h.matmul(psf[0:4, 0:GW], lhsT=wg[:, kc, 4:8], rhs=self.hT[:, kc, sl],
                                                             start=(kc == 0), stop=(kc == KC - 1)),
                             reads=[r_wg, self.hTr[g]], writes=[prf])
                    k.op("dve", lambda h: h.tensor_scalar(A[:, sl], psi[0:4, 0:GW], bcol[:, 0:1], None, op0=ALU.add),
                         reads=[pri, r_wg], writes=[r_rows])
                    k.op("act", lambda h: h.activation(out=Bt[:, sl], in_=psf[0:4, 0:GW], func=AF.Exp, scale=-1.0,
                                                       bias=nbf[:, 0:1]),
                         reads=[prf, r_wg], writes=[r_rows])
                k.op("act", lambda h: h.activation(out=Bt[:], in_=Bt[:], func=AF.Ln, bias=1.0),
                     reads=[r_rows], writes=[r_rows])
                k.op("dve", lambda h: h.tensor_tensor_scan(C[:], ones_r[:], Bt[:], 0.0, op0=ALU.mult, op1=ALU.add),
                     reads=[r_rows], writes=[r_rows])
                k.op("dve", lambda h: h.tensor_add(A[:], A[:], C[:]), reads=[r_rows], writes=[r_rows])
                k.op("dve", lambda h: h.tensor_tensor_scan(Bt[:], ones_r[:], A[:], 0.0, op0=ALU.mult, op1=ALU.max),
                     reads=[r_rows], writes=[r_rows])
                k.op("dve", lambda h: h.tensor_sub(C[:], C[:], Bt[:]), reads=[r_rows], writes=[r_rows])
                k.op("act", lambda h: h.activation(out=C[:], in_=C[:], func=AF.Exp), reads=[r_rows], writes=[r_rows])
                for src, dst in ((A, Bg_col), (Bt, G_col), (C, thr_col)):
                    pst, prt = self.psum()
                    for c in range(NT):
                        k.op("pe", lambda h, c=c, src=src: h.transpose(pst[:, c * H:(c + 1) * H],
                                                                       src[:, c * 128:(c + 1) * 128],
                                                                       self.ident_f[0:4, 0:4]),
                             reads=[r_rows, self.c_res], writes=[prt])
                    k.op("dve", lambda h, dst=dst: h.tensor_copy(dst[:].rearrange("p c h -> p (c h)"), pst[:, 0:NT * H]),
                         reads=[prt], writes=[r_cols])
                if self.debug and "gcols" in self.debug and b == 0:
                    dr = k.res("dbg_gcols")
                    for qi, t_ in enumerate((Bg_col, G_col, thr_col)):
                        k.dma("sp", self.dbg_d["gcols"][qi].rearrange("(c p) h -> p c h", p=128), t_[:],
                              reads=[r_cols], writes=[dr], sem_res=dr)
                    self.final_res = getattr(self, "final_res", []) + [dr]
                k.barrier()
            if self.stage <= 1:
                return
            G1b = self.sb([128, D], F32, "G1b", ls)
            r_G1 = k.res("G1b")
            self.bcast_row(self.mod_col(l, 2, b), G1b, r_G1, [self.mod_res])
            wout = self.sb([128, KC, D], BF16, "wout", ls)
            r_wout = k.res("wout")
            k.dma("pool", wout[:], self.a_w_out_d.rearrange("(kc p) n -> p kc n", p=128), writes=[r_wout], sem_res=r_wout)
            yT = self.sb([128, KC, S], BF16, "yT", ls)
            r_yT = [k.res("yT%d" % i) for i in range(NT)]
            wsl = [self.sb([128, KC, 768], BF16, "wsl", ls) for _ in range(2)]
            r_wsl = [k.res("wsl0"), k.res("wsl1")]

            def load_wsl(h_):
                bi = h_ % 2
                for (dst0, n, src0) in ((0, 128, h_ * 128), (128, 128, 512 + h_ * 128),
                                        (256, 256, 1024 + h_ * 256), (512, 256, 2048 + h_ * 256)):
                    k.dma("pool", wsl[bi][:, :, dst0:dst0 + n],
                          win[:, src0:src0 + n].rearrange("(kc p) n -> p kc n", p=128),
                          writes=[r_wsl[bi]], sem_res=r_wsl[bi])

            def two(shape, dt, nm):
                return [self.sb(shape, dt, nm, ls) for _ in range(2)], [k.res(nm + "0"), k.res(nm + "1")]

            qT, r_qT = two([128, GW], BF16, "qT")
            kT, r_kT = two([128, GW], BF16, "kT")
            ktok, r_ktok = two([128, 128], BF16, "ktok")
            vaug, r_vaug = two([128, 257], BF16, "vaug")
            sig, r_sig = two([128, 256], F32, "sig")
            diag, r_diag = two([128, 128], F32, "diag")
            z, r_z = two([128, 128], F32, "z")
            DT, r_DT = two([128, 128], F32, "DT")
            itb, r_itb = two([128, 128], F32, "itb")
            PT, r_PT = two([128, 128], BF16, "PT")
            QpT, r_QpT = two([128, 128], BF16, "QpT")
            kw, r_kw = two([128, 128], BF16, "kw")
            u, r_u = two([128, 256], F32, "u")
            ybf, r_ybf = two([128, 256], BF16, "ybf")
            junk2, r_junk2 = two([128, 256], BF16, "junk2")
            sc1, r_sc1 = two([128, 8], F32, "sc1")
            G0c, r_G0 = two([128, 1], F32, "G0c")
            C_f = self.sb([128, 257], F32, "C_f", ls)
            C_b = self.sb([128, 257], BF16, "C_b", ls)
            r_Cf, r_Cb = k.res("C_f"), k.res("C_b")
            mhalf = self.sb([128, 1], F32, "mhalf", ls)
            r_mh = k.res("mhalf")
            k.op("dve", lambda h: h.memset(mhalf[:], -0.5), writes=[r_mh])
            for i_ in range(2):
                k.op("dve", lambda h, i_=i_: h.memset(vaug[i_][:, 256:257], 1.0), writes=[r_vaug[i_]])
            load_wsl(0)
            SCALE = 128.0 ** -0.5
            if self.stage <= 1.2:
                k.barrier()
                return
            it = 0
            for hd in range(H):
                if hd + 1 < H:
                    load_wsl(hd + 1)
                wb, rw = wsl[hd % 2], r_wsl[hd % 2]
                k.op("dve", lambda h: h.memset(C_f[:], 0.0), writes=[r_Cf])
                k.op("dve", lambda h: h.memset(C_b[:], 0.0), writes=[r_Cb])
                k.op("dve", lambda h: h.memset(G0c[it % 2][:], 0.0), writes=[r_G0[it % 2]])
                for c in range(NT):
                    g = c // TPG
                    cg = c % TPG
                    p = it % 2
                    it += 1
                    tok = slice(c * 128, (c + 1) * 128)
                    tg = slice(cg * 128, (cg + 1) * 128)
                    qTg, rqT, kTg, rkT = qT[g % 2], r_qT[g % 2], kT[g % 2], r_kT[g % 2]
                    if cg == 0:
                        gs = slice(g * GW, (g + 1) * GW)
                        psq, prq = self.psum()
                        for kc in range(KC):
                            k.op("pe", lambda h, kc=kc: h.matmul(psq[:, 0:GW], lhsT=wb[:, kc, 0:128], rhs=self.hT[:, kc, gs],
                                                                 start=(kc == 0), stop=(kc == KC - 1)),
                                 reads=[rw, self.hTr[g]], writes=[prq])
                        psk, prk = self.psum()
                        for kc in range(KC):
                            k.op("pe", lambda h, kc=kc: h.matmul(psk[:, 0:GW], lhsT=wb[:, kc, 128:256], rhs=self.hT[:, kc, gs],
                                                                 start=(kc == 0), stop=(kc == KC - 1)),
                                 reads=[rw, self.hTr[g]], writes=[prk])
                        k.op("act", lambda h: h.activation(out=qTg[:], in_=psq[:, 0:GW], func=AF.Copy),
                             reads=[prq], writes=[rqT])
                        k.op("dve", lambda h: h.tensor_scalar(kTg[:], psk[:, 0:GW], SCALE, None, op0=ALU.mult),
                             reads=[prk], writes=[rkT])
                    if self.stage < 1.7:
                        continue
                    pskv, prkv = self.psum()
                    for kc in range(KC):
                        k.op("pe", lambda h, kc=kc: h.matmul(pskv[:, 0:384], lhsT=self.hT[:, kc, tok], rhs=wb[:, kc, 128:512],
                                                             start=(kc == 0), stop=(kc == KC - 1)),
                             reads=[rw, self.hTr[g]], writes=[prkv])
                    if self.stage < 1.72:
                        continue
                    pso, pro = self.psum()
                    for kc in range(KC):
                        k.op("pe", lambda h, kc=kc: h.matmul(pso[:, 0:256], lhsT=self.hT[:, kc, tok], rhs=wb[:, kc, 512:768],
                                                             start=(kc == 0), stop=(kc == KC - 1)),
                             reads=[rw, self.hTr[g]], writes=[pro])
                    if self.stage < 1.73:
                        continue
                    k.op("act", lambda h: h.activation(out=ktok[p][:], in_=pskv[:, 0:128], func=AF.Copy, scale=SCALE),
                         reads=[prkv], writes=[r_ktok[p]])
                    if self.stage < 1.74:
                        continue
                    k.op("act", lambda h: h.activation(out=vaug[p][:, 0:256], in_=pskv[:, 128:384], func=AF.Copy),
                         reads=[prkv], writes=[r_vaug[p]])
                    if self.stage < 1.75:
                        continue
                    k.op("act", lambda h: h.activation(out=sig[p][:], in_=pso[:, 0:256], func=AF.Exp, scale=-1.0),
                         reads=[pro], writes=[r_sig[p]])
                    k.op("dve", lambda h: h.tensor_scalar_add(sig[p][:], sig[p][:], 1.0), reads=[r_sig[p]], writes=[r_sig[p]])
                    k.op("dve", lambda h: h.reciprocal(sig[p][:], sig[p][:]), reads=[r_sig[p]], writes=[r_sig[p]])
                    if self.stage < 3:
                        continue
                    k.op("dve", lambda h: h.tensor_scalar(diag[p][:], self.ident_f[:], G_col[:, c, hd:hd + 1], None,
                                                          op0=ALU.mult),
                         reads=[r_cols, self.c_res], writes=[r_diag[p]])
                    psG, prG = self.psum()
                    k.op("pe", lambda h: h.matmul(psG[:, 0:128], lhsT=self.ones_f[:], rhs=diag[p][:], start=True, stop=True),
                         reads=[r_diag[p], self.c_res], writes=[prG])
                    k.op("dve", lambda h: h.scalar_tensor_tensor(z[p][:], psG[:, 0:128], -1.0, self.maskadd[:],
                                                                 op0=ALU.mult, op1=ALU.add),
                         reads=[prG, self.c_res], writes=[r_z[p]])
                    k.op("act", lambda h: h.activation(out=DT[p][:], in_=z[p][:], func=AF.Exp,
                                                       bias=Bg_col[:, c, hd:hd + 1]),
                         reads=[r_z[p], r_cols], writes=[r_DT[p]])
                    g0, rg0 = G0c[p], r_G0[p]
                    g0n, rg0n = G0c[1 - p], r_G0[1 - p]
                    k.op("act", lambda h: h.activation(out=itb[p][:], in_=psG[:, 0:128], func=AF.Exp, scale=-1.0,
                                                       bias=g0[:, 0:1]),
                         reads=[prG, rg0], writes=[r_itb[p]])
                    k.op("act", lambda h: h.activation(out=sc1[p][:, 0:1], in_=psG[:, 127:128], func=AF.Exp, scale=-1.0,
                                                       bias=Bg_col[:, c, hd:hd + 1]),
                         reads=[prG, r_cols], writes=[r_sc1[p]])
                    k.op("act", lambda h: h.activation(out=sc1[p][:, 1:2], in_=psG[:, 127:128], func=AF.Exp, scale=-1.0,
                                                       bias=g0[:, 0:1]),
                         reads=[prG, rg0], writes=[r_sc1[p]])
                    k.op("act", lambda h: h.activation(out=g0n[:], in_=psG[:, 127:128], func=AF.Copy),
                         reads=[prG], writes=[rg0n])
                    if self.stage < 4:
                        continue
                    psS, prS = self.psum()
                    k.op("pe", lambda h: h.matmul(psS[:, 0:128], lhsT=kTg[:, tg], rhs=qTg[:, tg], start=True, stop=True),
                         reads=[rkT, rqT], writes=[prS])
                    k.op("dve", lambda h: h.tensor_tensor(PT[p][:], psS[:, 0:128], DT[p][:], op=ALU.mult),
                         reads=[prS, r_DT[p]], writes=[r_PT[p]])
                    k.op("dve", lambda h: h.tensor_tensor(QpT[p][:], qTg[:, tg], itb[p][:], op=ALU.mult),
                         reads=[rqT, r_itb[p]], writes=[r_QpT[p]])
                    if self.stage < 5:
                        continue
                    psN, prN = self.psum()
                    k.op("pe", lambda h: h.matmul(psN[:, 0:257], lhsT=PT[p][:], rhs=vaug[p][:], start=True, stop=False),
                         reads=[r_PT[p], r_vaug[p]], writes=[prN])
                    k.op("pe", lambda h: h.matmul(psN[:, 0:257], lhsT=QpT[p][:], rhs=C_b[:], start=False, stop=True),
                         reads=[r_QpT[p], r_Cb], writes=[prN])
                    k.op("dve", lambda h: h.tensor_reduce(sc1[p][:, 2:3], psN[:, 256:257], axis=AX.X, op=ALU.max,
                                                          apply_absolute_value=True),
                         reads=[prN], writes=[r_sc1[p]])
                    k.op("dve", lambda h: h.tensor_scalar(sc1[p][:, 2:3], sc1[p][:, 2:3], thr_col[:, c, hd:hd + 1], None,
                                                          op0=ALU.max),
                         reads=[r_sc1[p], r_cols], writes=[r_sc1[p]])
                    k.op("dve", lambda h: h.reciprocal(sc1[p][:, 2:3], sc1[p][:, 2:3]), reads=[r_sc1[p]], writes=[r_sc1[p]])
                    k.op("act", lambda h: h.activation(out=u[p][:], in_=psN[:, 0:256], func=AF.Copy, scale=sc1[p][:, 2:3]),
                         reads=[prN, r_sc1[p]], writes=[r_u[p]])
                    k.op("act", lambda h: h.activation(out=junk2[p][:], in_=u[p][:], func=AF.Square,
                                                       accum_out=sc1[p][:, 3:4]),
                         reads=[r_u[p]], writes=[r_junk2[p], r_sc1[p]])
                    k.op("dve", lambda h: h.tensor_scalar(sc1[p][:, 4:5], sc1[p][:, 3:4], 1.0 / 256, EPS,
                                                          op0=ALU.mult, op1=ALU.add),
                         reads=[r_sc1[p]], writes=[r_sc1[p]])
                    k.op("act", lambda h: h.activation(out=sc1[p][:, 4:5], in_=sc1[p][:, 4:5], func=AF.Ln),
                         reads=[r_sc1[p]], writes=[r_sc1[p]])
                    k.op("act", lambda h: h.activation(out=sc1[p][:, 4:5], in_=sc1[p][:, 4:5], func=AF.Exp, scale=-0.5),
                         reads=[r_sc1[p]], writes=[r_sc1[p]])
                    k.op("dve", lambda h: h.scalar_tensor_tensor(ybf[p][:], u[p][:], sc1[p][:, 4:5], sig[p][:],
                                                                 op0=ALU.mult, op1=ALU.mult),
                         reads=[r_u[p], r_sc1[p], r_sig[p]], writes=[r_ybf[p]])
                    psT, prT = self.psum()
                    psTb = psT[:].bitcast(BF16)
                    for j in range(2):
                        k.op("pe", lambda h, j=j: h.transpose(psTb[:, j * 128:(j + 1) * 128], ybf[p][:, j * 128:(j + 1) * 128],
                                                              self.ident_b[:]),
                             reads=[r_ybf[p], self.c_res], writes=[prT])
                    for j in range(2):
                        fcol = 128 + hd * 2 + j
                        k.op("act", lambda h, j=j, fcol=fcol: h.activation(
                            out=yT[:, hd * 2 + j, tok], in_=psTb[:, j * 128:(j + 1) * 128], func=AF.Copy,
                            scale=self.vecT[:, fcol:fcol + 1]),
                            reads=[prT, self.c_res], writes=[r_yT[c]])
                    if self.stage < 6:
                        continue
                    k.op("dve", lambda h: h.tensor_scalar(kw[p][:], ktok[p][:], sc1[p][:, 0:1], None, op0=ALU.mult),
                         reads=[r_ktok[p], r_sc1[p]], writes=[r_kw[p]])
                    psC, prC = self.psum()
                    k.op("pe", lambda h: h.matmul(psC[:, 0:257], lhsT=kw[p][:], rhs=vaug[p][:], start=True, stop=True),
                         reads=[r_kw[p], r_vaug[p]], writes=[prC])
                    k.op("dve", lambda h: h.scalar_tensor_tensor(C_f[:], C_f[:], sc1[p][:, 1:2], psC[:, 0:257],
                                                                 op0=ALU.mult, op1=ALU.add),
                         reads=[r_Cf, r_sc1[p], prC], writes=[r_Cf])
                    k.op("act", lambda h: h.activation(out=C_b[:], in_=C_f[:], func=AF.Copy),
                         reads=[r_Cf], writes=[r_Cb])
            tmp, r_tmp = two([128, 512], F32, "tmpo")
            cnt = 0
            for i in range(NT):
                tok = slice(i * 128, (i + 1) * 128)
                for dh in range(2):
                    pso, pro = self.psum()
                    for j in range(KC):
                        k.op("pe", lambda h, j=j: h.matmul(pso[:], lhsT=yT[:, j, tok], rhs=wout[:, j, dh * 512:(dh + 1) * 512],
                                                           start=(j == 0), stop=(j == KC - 1)),
                             reads=[r_yT[i], r_wout], writes=[pro])
                    t_, rt_ = tmp[cnt % 2], r_tmp[cnt % 2]
                    cnt += 1
                    k.op("dve", lambda h, t_=t_: h.tensor_tensor(t_[:], pso[:], G1b[:, dh * 512:(dh + 1) * 512], op=ALU.mult),
                         reads=[pro, r_G1], writes=[rt_])
                    k.op("pool", lambda h, t_=t_: h.tensor_tensor(
                        self.x_sb[:, i, dh * 512:(dh + 1) * 512], self.x_sb[:, i, dh * 512:(dh + 1) * 512], t_[:], op=ALU.add),
                        reads=[rt_, self.xr[i]], writes=[self.xr[i]])
            k.barrier()

    def diffattn(self, b, l):
        k, nc, NT, S = self.k, self.nc, self.NT, self.S
        win = self.b_w_in_d
        lam_init = 0.8 - 0.6 * math.exp(-0.3 * l)
        PI = math.pi
        with ExitStack() as ls:
            r_pre = k.res("attn_pre")
            lamt = self.sb([128, 8], F32, "lamt", ls)
            prod = self.sb([128, 64], F32, "prod", ls)
            sb_ = self.small_b
            for ci, (o1, o2) in enumerate(((136, 200), (264, 328))):
                k.op("dve", lambda h, o1=o1, o2=o2: h.tensor_mul(prod[:], sb_[:, o1:o1 + 64], sb_[:, o2:o2 + 64]),
                     reads=[self.c_res, r_pre], writes=[r_pre])
                k.op("dve", lambda h, ci=ci: h.tensor_reduce(lamt[:, ci:ci + 1], prod[:], axis=AX.X, op=ALU.add),
                     reads=[r_pre], writes=[r_pre])
            k.op("act", lambda h: h.activation(out=lamt[:, 0:2], in_=lamt[:, 0:2], func=AF.Exp), reads=[r_pre], writes=[r_pre])
            k.op("dve", lambda h: h.scalar_tensor_tensor(lamt[:, 2:3], lamt[:, 1:2], -lam_init, lamt[:, 0:1],
                                                         op0=ALU.add, op1=ALU.subtract),
                 reads=[r_pre], writes=[r_pre])
            cosT = self.sb([128, NT, 8], F32, "cosT", ls)
            sinT = self.sb([128, NT, 8], F32, "sinT", ls)
            with ExitStack() as ps_:
                posf = self.sb([128, NT], F32, "posf", ps_)
                invf = self.sb([128, 8], F32, "invf", ps_)
                ang = self.sb([128, NT, 8], F32, "ang", ps_)
                qi = self.sb([128, NT, 8], I32, "qi", ps_)
                qf = self.sb([128, NT, 8], F32, "qf", ps_)
                mk = self.sb([128, NT, 8], F32, "mk", ps_)
                r2 = self.sb([128, NT, 8], F32, "r2", ps_)
                dv = lambda fn: k.op("dve", fn, reads=[r_pre, self.c_res], writes=[r_pre])
                dv(lambda h: h.tensor_copy(posf[:], self.posT[:, b * NT:(b + 1) * NT]))
                for i in range(8):
                    dv(lambda h, i=i: h.memset(invf[:, i:i + 1], float(np.float32(ROPE_THETA) ** np.float32(-2.0 * i / 16))))
                dv(lambda h: h.tensor_tensor(ang[:], posf[:].unsqueeze(2).broadcast_to([128, NT, 8]),
                                             invf[:].unsqueeze(1).broadcast_to([128, NT, 8]), op=ALU.mult))
                dv(lambda h: h.tensor_scalar(qf[:], ang[:], 1.0 / (2 * PI), None, op0=ALU.mult))
                dv(lambda h: h.tensor_copy(qi[:], qf[:]))
                dv(lambda h: h.tensor_copy(qf[:], qi[:]))
                C1 = 6.28125
                C2 = 2 * PI - C1
                dv(lambda h: h.scalar_tensor_tensor(ang[:], qf[:], -C1, ang[:], op0=ALU.mult, op1=ALU.add))
                dv(lambda h: h.scalar_tensor_tensor(ang[:], qf[:], -C2, ang[:], op0=ALU.mult, op1=ALU.add))

                def fold(t):
                    dv(lambda h: h.tensor_single_scalar(mk[:], t[:], PI, op=ALU.is_gt))
                    dv(lambda h: h.scalar_tensor_tensor(t[:], mk[:], -2 * PI, t[:], op0=ALU.mult, op1=ALU.add))
                    dv(lambda h: h.tensor_single_scalar(mk[:], t[:], -PI, op=ALU.is_lt))
                    dv(lambda h: h.scalar_tensor_tensor(t[:], mk[:], 2 * PI, t[:], op0=ALU.mult, op1=ALU.add))
                    dv(lambda h: h.tensor_scalar(t[:], t[:], -PI, PI, op0=ALU.max, op1=ALU.min))

                fold(ang)
                dv(lambda h: h.tensor_scalar_add(r2[:], ang[:], PI / 2))
                fold(r2)
                k.op("act", lambda h: h.activation(out=sinT[:], in_=ang[:], func=AF.Sin), reads=[r_pre], writes=[r_pre])
                k.op("act", lambda h: h.activation(out=cosT[:], in_=r2[:], func=AF.Sin), reads=[r_pre], writes=[r_pre])
                k.barrier()
            if self.stage <= 1.5:
                return
            G1b = self.sb([128, D], F32, "G1b", ls)
            r_G1 = k.res("G1b")
            self.bcast_row(self.mod_col(l, 2, b), G1b, r_G1, [self.mod_res])
            HH = 4
            qT = self.sb([128, HH, S], BF16, "qT", ls)
            kT = self.sb([128, HH, S], BF16, "kT", ls)
            vaug = self.sb([128, NT, HH, 130], BF16, "vaug", ls)
            r_q = [k.res("qTt%d" % i) for i in range(NT)]
            r_k = [k.res("kTt%d" % i) for i in range(NT)]
            r_v = [k.res("vt%d" % i) for i in range(NT)]

            def two(shape, dt, nm, st_):
                return [self.sb(shape, dt, nm, st_) for _ in range(2)], [k.res(nm + "0"), k.res(nm + "1")]

            for half in range(2):
                with ExitStack() as sa:
                    gains = self.sb([128, 16, 64], F32, "gains", sa)
                    r_gains = k.res("gains")
                    for gi in range(16):
                        off = 8 if gi < 8 else 72
                        k.op("dve", lambda h, gi=gi, off=off: h.tensor_copy(gains[:, gi, :], sb_[:, off:off + 64]),
                             reads=[self.c_res], writes=[r_gains])
                    wqkv = self.sb([128, KC, 1536], BF16, "wqkv", sa)
                    r_w = k.res("wqkv")
                    for pi_ in range(3):
                        k.dma("pool", wqkv[:, :, pi_ * 512:(pi_ + 1) * 512],
                              win[:, pi_ * 1024 + half * 512: pi_ * 1024 + (half + 1) * 512].rearrange("(kc p) n -> p kc n", p=128),
                              writes=[r_w], sem_res=r_w)
                    for i in range(NT):
                        k.op("pool", lambda h, i=i: h.memset(vaug[:, i, :, 128:129], 1.0), writes=[r_v[i]])
                    sq, r_sq = two([128, 16, 64], F32, "sq", sa)
                    qn, r_qn = two([128, 16, 64], F32, "qn", sa)
                    qkb, r_qkb = two([128, 16, 64], BF16, "qkb", sa)
                    st_, r_st = two([128, 32], F32, "stat", sa)
                    tA, r_tA = two([128, 16, 8], F32, "tA", sa)
                    tB, r_tB = two([128, 16, 8], F32, "tB", sa)
                    def tile_gen(i, p):
                        g = (i * 128) // self.GW
                        tok = slice(i * 128, (i + 1) * 128)
                        pss = []
                        for pi_ in range(3):
                            ps, pr = self.psum()
                            for kc in range(KC):
                                k.op("pe", lambda h, kc=kc, ps=ps, pi_=pi_: h.matmul(
                                    ps[:], lhsT=self.hT[:, kc, tok], rhs=wqkv[:, kc, pi_ * 512:(pi_ + 1) * 512],
                                    start=(kc == 0), stop=(kc == KC - 1)),
                                    reads=[r_w, self.hTr[g]], writes=[pr])
                            pss.append((ps, pr))
                        (psq, prq), (psk, prk), (psv, prv) = pss
                        k.op("act", lambda h: h.activation(out=vaug[:, i, :, 0:128], in_=psv[:].rearrange("p (a d) -> p a d", a=HH),
                                                           func=AF.Copy),
                             reads=[prv], writes=[r_v[i]])
                        qflat_f = qn[p][:].rearrange("p a d -> p (a d)")
                        k.op("act", lambda h: h.activation(out=qflat_f[:, 0:512], in_=psq[:], func=AF.Copy),
                             reads=[prq], writes=[r_qn[p]])
                        k.op("act", lambda h: h.activation(out=qflat_f[:, 512:1024], in_=psk[:], func=AF.Copy),
                             reads=[prk], writes=[r_qn[p]])
                        yield
                        k.op("act", lambda h: h.activation(out=sq[p][:], in_=qn[p][:], func=AF.Square),
                             reads=[r_qn[p]], writes=[r_sq[p]])
                        yield
                        k.op("dve", lambda h: h.tensor_reduce(st_[p][:, 0:16], sq[p][:], axis=AX.X, op=ALU.add),
                             reads=[r_sq[p]], writes=[r_st[p]])
                        k.op("dve", lambda h: h.tensor_scalar(st_[p][:, 16:32], st_[p][:, 0:16], 1.0 / 64, EPS,
                                                              op0=ALU.mult, op1=ALU.add), reads=[r_st[p]], writes=[r_st[p]])
                        yield
                        k.op("act", lambda h: h.activation(out=st_[p][:, 16:32], in_=st_[p][:, 16:32], func=AF.Ln),
                             reads=[r_st[p]], writes=[r_st[p]])
                        k.op("act", lambda h: h.activation(out=st_[p][:, 16:32], in_=st_[p][:, 16:32], func=AF.Exp, scale=-0.5),
                             reads=[r_st[p]], writes=[r_st[p]])
                        yield
                        k.op("dve", lambda h: h.tensor_scalar(st_[p][:, 16:24], st_[p][:, 16:24], 0.125, None, op0=ALU.mult),
                             reads=[r_st[p]], writes=[r_st[p]])
                        k.op("dve", lambda h: h.tensor_tensor(
                            qn[p][:], qn[p][:], st_[p][:, 16:32].unsqueeze(2).broadcast_to([128, 16, 64]), op=ALU.mult),
                            reads=[r_qn[p], r_st[p]], writes=[r_qn[p]])
                        yield
                        k.op("pool", lambda h: h.tensor_tensor(qn[p][:], qn[p][:], gains[:], op=ALU.mult),
                             reads=[r_qn[p], r_gains], writes=[r_qn[p]])
                        yield
                        k.op("act", lambda h: h.activation(out=qkb[p][:, :, 16:64], in_=qn[p][:, :, 16:64], func=AF.Copy),
                             reads=[r_qn[p]], writes=[r_qkb[p]])
                        cb = cosT[:, i, :].unsqueeze(1).broadcast_to([128, 16, 8])
                        sbb = sinT[:, i, :].unsqueeze(1).broadcast_to([128, 16, 8])
                        x1 = qn[p][:, :, 0:8]
                        x2 = qn[p][:, :, 8:16]
                        k.op("dve", lambda h: h.tensor_tensor(tA[p][:], x1, cb, op=ALU.mult), reads=[r_qn[p], r_pre], writes=[r_tA[p]])
                        k.op("dve", lambda h: h.tensor_tensor(tB[p][:], x2, sbb, op=ALU.mult), reads=[r_qn[p], r_pre], writes=[r_tB[p]])
                        yield
                        k.op("dve", lambda h: h.tensor_tensor(qkb[p][:, :, 0:8], tA[p][:], tB[p][:], op=ALU.subtract),
                             reads=[r_tA[p], r_tB[p]], writes=[r_qkb[p]])
                        k.op("dve", lambda h: h.tensor_tensor(tA[p][:], x2, cb, op=ALU.mult), reads=[r_qn[p], r_pre], writes=[r_tA[p]])
                        k.op("dve", lambda h: h.tensor_tensor(tB[p][:], x1, sbb, op=ALU.mult), reads=[r_qn[p], r_pre], writes=[r_tB[p]])
                        yield
                        k.op("dve", lambda h: h.tensor_tensor(qkb[p][:, :, 8:16], tA[p][:], tB[p][:], op=ALU.add),
                             reads=[r_tA[p], r_tB[p]], writes=[r_qkb[p]])
                        yield
                        pst, prt = self.psum()
                        pstb = pst[:].bitcast(BF16)
                        qflat = qkb[p][:].rearrange("p a d -> p (a d)")
                        for j in range(8):
                            k.op("pe", lambda h, j=j: h.transpose(pstb[:, j * 128:(j + 1) * 128], qflat[:, j * 128:(j + 1) * 128],
                                                                  self.ident_b[:]),
                                 reads=[r_qkb[p], self.c_res], writes=[prt])
                        for a_ in range(HH):
                            k.op("dve", lambda h, a_=a_: h.tensor_scalar(qT[:, a_, tok], pstb[:, a_ * 128:(a_ + 1) * 128],
                                                                       1.0, None, op0=ALU.mult),
                                 reads=[prt], writes=[r_q[i]])
                            k.op("dve", lambda h, a_=a_: h.tensor_scalar(kT[:, a_, tok], pstb[:, 512 + a_ * 128:512 + (a_ + 1) * 128],
                                                                       1.0, None, op0=ALU.mult),
                                 reads=[prt], writes=[r_k[i]])

                    self.run_interleaved([(i,) for i in range(NT)], 2, tile_gen)
                    k.barrier()
                if self.stage < 3:
                    continue
                with ExitStack() as sbk:
                    oT = self.sb([128, HH, S], BF16, "oT", sbk)
                    r_oT = [k.res("oT%d" % i) for i in range(NT)]
                    wo = self.sb([128, HH, D], BF16, "wo", sbk)
                    r_wo = k.res("wo")
                    k.dma("pool", wo[:], self.b_w_out_d[half * 512:(half + 1) * 512, :].rearrange("(kc p) n -> p kc n", p=128),
                          writes=[r_wo], sem_res=r_wo)
                    W = 4

                    def nset(shape, dt, nm):
                        return [self.sb(shape, dt, nm, sbk) for _ in range(W)], [k.res("%s%d" % (nm, i_)) for i_ in range(W)]

                    pTs, r_pTs = nset([128, NT, 128], BF16, "pT")
                    t0, r_t0 = nset([128, 128], F32, "t0")
                    of, r_of = nset([128, 128], F32, "of")
                    ob, r_ob = nset([128, 128], BF16, "ob")
                    jk, r_jk = nset([128, 128], BF16, "jk")
                    sc, r_sc = nset([128, 8], F32, "sc")

                    def attn_iter(hl, j, p):
                        nkb = j + 1
                        qs = slice(j * 128, (j + 1) * 128)
                        pT, r_pT = pTs[p], r_pTs[p]
                        for m in range(2):
                            rows = slice(m * 64, (m + 1) * 64)
                            for kg in range(0, nkb, 4):
                                n = min(4, nkb - kg)
                                ps, pr = self.psum()
                                for idx in range(n):
                                    kb = kg + idx
                                    k.op("pe", lambda h, idx=idx, kb=kb: h.matmul(
                                        ps[:, idx * 128:(idx + 1) * 128], lhsT=kT[rows, hl, kb * 128:(kb + 1) * 128],
                                        rhs=qT[rows, hl, qs], start=True, stop=True),
                                        reads=[r_k[kb], r_q[j]], writes=[pr])
                                k.op("act", lambda h, kg=kg, n=n: h.activation(
                                    out=pT[:, kg:kg + n, :].rearrange("p a t -> p (a t)"), in_=ps[:, 0:n * 128],
                                    func=AF.Exp), reads=[pr], writes=[r_pT])
                                yield
                            k.op("dve", lambda h: h.tensor_tensor(pT[:, j, :], pT[:, j, :], self.mask01[:], op=ALU.mult),
                                 reads=[r_pT, self.c_res], writes=[r_pT])
                            yield
                            acc, ar = self.psum()
                            for kb in range(nkb):
                                k.op("pe", lambda h, kb=kb: h.matmul(
                                    acc[:, 0:129], lhsT=pT[:, kb, :], rhs=vaug[:, kb, hl, 0:129],
                                    start=(kb == 0), stop=(kb == nkb - 1)),
                                    reads=[r_pT, r_v[kb]], writes=[ar])
                            k.op("dve", lambda h: h.reciprocal(sc[p][:, m:m + 1], acc[:, 128:129]), reads=[ar], writes=[r_sc[p]])
                            if m == 0:
                                k.op("act", lambda h: h.activation(out=t0[p][:], in_=acc[:, 0:128], func=AF.Copy, scale=sc[p][:, 0:1]),
                                     reads=[ar, r_sc[p]], writes=[r_t0[p]])
                            else:
                                k.op("dve", lambda h: h.tensor_scalar(sc[p][:, 1:2], sc[p][:, 1:2], lamt[:, 2:3], None, op0=ALU.mult),
                                     reads=[r_sc[p], r_pre], writes=[r_sc[p]])
                                k.op("dve", lambda h: h.scalar_tensor_tensor(of[p][:], acc[:, 0:128], sc[p][:, 1:2], t0[p][:],
                                                                             op0=ALU.mult, op1=ALU.add),
                                     reads=[ar, r_sc[p], r_t0[p]], writes=[r_of[p]])
                            yield
                        k.op("act", lambda h: h.activation(out=jk[p][:], in_=of[p][:], func=AF.Square, accum_out=sc[p][:, 2:3]),
                             reads=[r_of[p]], writes=[r_jk[p], r_sc[p]])
                        yield
                        k.op("dve", lambda h: h.tensor_scalar(sc[p][:, 3:4], sc[p][:, 2:3], 1.0 / 128, EPS, op0=ALU.mult, op1=ALU.add),
                             reads=[r_sc[p]], writes=[r_sc[p]])
                        yield
                        k.op("act", lambda h: h.activation(out=sc[p][:, 3:4], in_=sc[p][:, 3:4], func=AF.Ln),
                             reads=[r_sc[p]], writes=[r_sc[p]])
                        k.op("act", lambda h: h.activation(out=sc[p][:, 3:4], in_=sc[p][:, 3:4], func=AF.Exp, scale=-0.5),
                             reads=[r_sc[p]], writes=[r_sc[p]])
                        yield
                        k.op("dve", lambda h: h.tensor_scalar(ob[p][:], of[p][:], sc[p][:, 3:4], 1.0 - lam_init,
                                                              op0=ALU.mult, op1=ALU.mult),
                             reads=[r_of[p], r_sc[p]], writes=[r_ob[p]])
                        yield
                        pst, prt = self.psum()
                        pstb = pst[:].bitcast(BF16)
                        k.op("pe", lambda h: h.transpose(pstb[:, 0:128], ob[p][:], self.ident_b[:]),
                             reads=[r_ob[p], self.c_res], writes=[prt])
                        k.op("act", lambda h: h.activation(out=oT[:, hl, qs], in_=pstb[:, 0:128], func=AF.Copy,
                                                           scale=self.onorm_col[:, 0:1]),
                             reads=[prt, self.c_res], writes=[r_oT[j]])

                    work = [(hl, j) for hl in range(HH) for j in reversed(range(NT))]
                    self.run_interleaved(work, W, attn_iter)
                    tmp, r_tmp = two([128, 512], F32, "tmpo", sbk)
                    cnt = 0
                    for i in range(NT):
                        tok = slice(i * 128, (i + 1) * 128)
                        for dh in range(2):
                            pso, pro = self.psum()
                            for jx in range(HH):
                                k.op("pe", lambda h, jx=jx: h.matmul(pso[:], lhsT=oT[:, jx, tok], rhs=wo[:, jx, dh * 512:(dh + 1) * 512],
                                                                     start=(jx == 0), stop=(jx == HH - 1)),
                                     reads=[r_oT[i], r_wo], writes=[pro])
                            t_, rt_ = tmp[cnt % 2], r_tmp[cnt % 2]
                            cnt += 1
                            k.op("dve", lambda h, t_=t_: h.tensor_tensor(t_[:], pso[:], G1b[:, dh * 512:(dh + 1) * 512], op=ALU.mult),
                                 reads=[pro, r_G1], writes=[rt_])
                            k.op("pool", lambda h, t_=t_: h.tensor_tensor(
                                self.x_sb[:, i, dh * 512:(dh + 1) * 512], self.x_sb[:, i, dh * 512:(dh + 1) * 512], t_[:], op=ALU.add),
                                reads=[rt_, self.xr[i]], writes=[self.xr[i]])
                    k.barrier()


def host_inputs(inp, core, NB, S):
    b0 = core * NB
    NT = S // 128
    vec = np.zeros((128, Prog.NV), np.float32)
    for l in range(DEPTH):
        vec[:, l * 64:l * 64 + 48] = inp["b_ada"][l].reshape(48, 128).T
        vec[:, l * 64 + 48:l * 64 + 56] = inp["norm1"][l].reshape(8, 128).T
        vec[:, l * 64 + 56:l * 64 + 64] = inp["norm2"][l].reshape(8, 128).T
    vec[:, 128:136] = inp["a_h_norm"][0].reshape(8, 128).T
    c = inp["c"][b0:b0 + NB]
    cT = np.ascontiguousarray(c.reshape(NB, KC, 128).transpose(2, 1, 0).reshape(128, KC * NB))
    pos = inp["positions"][b0:b0 + NB].reshape(NB, NT, 128)
    posT = np.ascontiguousarray(pos.transpose(2, 0, 1).reshape(128, NB * NT)).astype(np.int32)
    small = np.concatenate([inp["a_b_if"][0], inp["b_q_norm"][0], inp["b_k_norm"][0], inp["b_lam_q1"][0],
                            inp["b_lam_k1"][0], inp["b_lam_q2"][0], inp["b_lam_k2"][0],
                            inp["b_o_norm"][0]]).astype(np.float32)[None, :]
    return {
        "x": np.ascontiguousarray(inp["x"][b0:b0 + NB]),
        "cT": cT, "vecT": vec, "posT": posT,
        "w_ada": inp["w_ada"], "a_w_in": inp["a_w_in"][0], "a_w_out": inp["a_w_out"][0],
        "b_w_in": inp["b_w_in"][0], "b_w_out": inp["b_w_out"][0],
        "w_router": inp["w_router"], "rbias": inp["router_bias"][None, :].astype(np.float32),
        "small": small, "moe_w_gu": inp["moe_w_gu"], "moe_w_down": inp["moe_w_down"],
    }


def kernel(**inputs):
    inp = {k_: np.asarray(v) for k_, v in inputs.items()}
    NB = BATCH // N_CORES
    prog = Prog(S=SEQ, NB=NB)
    nc = prog.build()
    in_maps = [host_inputs(inp, c, NB, SEQ) for c in range(N_CORES)]
    res = run_bass_kernel_spmd(nc, in_maps, core_ids=list(range(N_CORES)))
    out = np.concatenate([r["out"] for r in res.results], axis=0)
    return out.astype(np.float32)
```

```python
import math
from contextlib import ExitStack

import numpy as np
import concourse.bass as bass
import concourse.mybir as mybir
from concourse.bass_utils import run_bass_kernel_spmd

F32 = mybir.dt.float32
BF16 = mybir.dt.bfloat16
I32 = mybir.dt.int32
AF = mybir.ActivationFunctionType
ALU = mybir.AluOpType
AX = mybir.AxisListType

D = 1024
KC = 8
N_CORES = 8
SEQ = 2048
BATCH = 16
DEPTH = 2
NE = 16
DE = 512
EPS = 1e-6
A_HEADS = 4
B_HEADS = 8
ROPE_THETA = 500000.0


class Res:
    __slots__ = ("name", "w", "r", "dsem")

    def __init__(self, name):
        self.name = name
        self.w = {}
        self.r = {}
        self.dsem = None


class Eng:
    def __init__(self, name, h, semi):
        self.name = name
        self.h = h
        self.semi = semi
        self.n = 0
        self.seen = {}


class K:
    def __init__(self, nc, stack):
        self.nc = nc
        self.stack = stack
        self.semh = []
        self.dma_tot = {}
        self.engs = {}
        for name, h in (("pe", nc.tensor), ("act", nc.scalar), ("dve", nc.vector),
                        ("pool", nc.gpsimd), ("sp", nc.sync)):
            self.engs[name] = Eng(name, h, self._newsem("e_" + name))
        self.n_ins = 0

    def _newsem(self, name):
        s = self.stack.enter_context(self.nc.semaphore("%s_%d" % (name, len(self.semh))))
        self.semh.append(s)
        return len(self.semh) - 1

    def res(self, name):
        return Res(name)

    def _gather(self, e, reads, writes):
        need = {}
        own = e.semi
        for r in reads:
            for s, v in r.w.items():
                if s == own and e.name == "pe":
                    continue
                if need.get(s, 0) < v:
                    need[s] = v
        for w in writes:
            for s, v in w.w.items():
                if s == own and e.name == "pe":
                    continue
                if need.get(s, 0) < v:
                    need[s] = v
            for s, v in w.r.items():
                if s == own and e.name == "pe":
                    continue
                if need.get(s, 0) < v:
                    need[s] = v
        for s in list(need):
            if s in self.dma_tot:
                need[s] = self.dma_tot[s]
        for s, v in need.items():
            if e.seen.get(s, 0) < v:
                e.h.wait_ge(self.semh[s], v)
                e.seen[s] = v

    def _record(self, ev, reads, writes):
        s, v = ev
        for w in writes:
            w.w = {s: v}
            w.r = {}
        for r in reads:
            if r.r.get(s, 0) < v:
                r.r[s] = v

    def op(self, en, fn, reads=(), writes=()):
        e = self.engs[en]
        self._gather(e, reads, writes)
        ins = fn(e.h)
        e.n += 1
        ins.then_inc(self.semh[e.semi], 1)
        self._record((e.semi, e.n), reads, writes)
        self.n_ins += 1
        return ins

    def dma(self, qn, out, in_, reads=(), writes=(), sem_res=None, **kw):
        e = self.engs[qn]
        self._gather(e, reads, writes)
        if sem_res.dsem is None:
            sem_res.dsem = {}
        kind = "sw" if qn == "pool" else "hw"
        if kind not in sem_res.dsem:
            sem_res.dsem[kind] = self._newsem("d%s_%s" % (kind, sem_res.name))
            self.dma_tot[sem_res.dsem[kind]] = 0
        s = sem_res.dsem[kind]
        ins = e.h.dma_start(out=out, in_=in_, **kw)
        self.dma_tot[s] += 16
        ins.then_inc(self.semh[s], 16)
        self._record((s, self.dma_tot[s]), reads, writes)
        self.n_ins += 1
        return ins

    def wait_res(self, en, res_list):
        self._gather(self.engs[en], res_list, [])

    def barrier(self):
        tgt = {e.semi: e.n for e in self.engs.values() if e.n > 0}
        for s, v in self.dma_tot.items():
            if v > 0:
                tgt[s] = v
        for e in self.engs.values():
            for s, v in tgt.items():
                if s == e.semi and e.name == "pe":
                    continue
                if e.seen.get(s, 0) < v:
                    e.h.wait_ge(self.semh[s], v)
                    e.seen[s] = v

    def final_wait(self, res_list):
        e = self.engs["sp"]
        need = {}
        for r in res_list:
            for s, v in list(r.w.items()) + list(r.r.items()):
                need[s] = max(need.get(s, 0), v)
        for s in list(need):
            if s in self.dma_tot:
                need[s] = self.dma_tot[s]
        for s, v in need.items():
            e.h.wait_ge(self.semh[s], v)


class Prog:
    def __init__(self, S=SEQ, NB=2, debug=None, layers=(0, 1), do_mixer=True, do_moe=True, stage=99):
        self.stage = stage
        self.S = S
        self.NB = NB
        self.NT = S // 128
        self.NG = max(1, S // 512)
        self.GW = min(512, S)
        self.debug = debug
        self.layers = layers
        self.do_mixer = do_mixer
        self.do_moe = do_moe
        self.nc = bass.Bass("TRN2", target_bir_lowering=False)
        self.stack = ExitStack()
        self._uid = 0

    def sb(self, shape, dt, name=None, stack=None):
        if getattr(self, "trace_alloc", False):
            print("ALLOC", name, shape, dt, "remaining", self.nc.sbuf_bytes_remaining)
        self._uid += 1
        nm = "%s_%d" % (name or "t", self._uid)
        st = stack if stack is not None else self.stack
        return st.enter_context(self.nc.sbuf_tensor(nm, list(shape), dt))

    def dram_in(self, name, shape, dt=F32):
        return self.nc.dram_tensor(name, list(shape), dt, kind="ExternalInput").ap()

    def run_interleaved(self, work, W, body):
        work = list(work)
        free = list(range(W))
        active = []
        wi = 0
        while wi < len(work) or active:
            while free and wi < len(work):
                slot = free.pop(0)
                active.append((slot, body(*work[wi], slot)))
                wi += 1
            nxt = []
            for slot, g_ in active:
                try:
                    next(g_)
                    nxt.append((slot, g_))
                except StopIteration:
                    free.append(slot)
            active = nxt

    def psum(self):
        i = self.ps_i
        self.ps_i = (i + 1) % len(self.ps_tiles)
        return self.ps_tiles[i], self.ps_res[i]

    def build(self):
        nc, S, NB, NT = self.nc, self.S, self.NB, self.NT
        st = self.stack
        k = self.k = K(nc, st)
        self.x_d = self.dram_in("x", [NB, S, D])
        self.out_d = nc.dram_tensor("out", [NB, S, D], F32, kind="ExternalOutput").ap()
        self.cT_d = self.dram_in("cT", [128, KC * NB])
        self.vecT_d = self.dram_in("vecT", [128, self.NV])
        self.pos_d = self.dram_in("posT", [128, NB * NT], I32)
        self.w_ada_d = self.dram_in("w_ada", [DEPTH, D, 6 * D])
        self.a_w_in_d = self.dram_in("a_w_in", [D, 3080])
        self.a_w_out_d = self.dram_in("a_w_out", [D, D])
        self.b_w_in_d = self.dram_in("b_w_in", [D, 3 * D])
        self.b_w_out_d = self.dram_in("b_w_out", [D, D])
        self.w_router_d = self.dram_in("w_router", [D, NE])
        self.rbias_d = self.dram_in("rbias", [1, NE])
        self.small_d = self.dram_in("small", [1, self.NSMALL])
        self.w_gu_d = self.dram_in("moe_w_gu", [DEPTH, NE, D, 2 * DE])
        self.w_dn_d = self.dram_in("moe_w_down", [DEPTH, NE, DE, D])
        if self.debug:
            self.dbg_d = {nm: nc.dram_tensor("dbg_" + nm, list(shp), F32, kind="ExternalOutput").ap()
                          for nm, shp in self.debug.items()}

        self.x_sb = self.sb([128, NT, D], F32, "x")
        self.xr = [k.res("x%d" % i) for i in range(NT)]
        self.hT = self.sb([128, KC, S], BF16, "hT")
        self.hTr = [k.res("hT%d" % g) for g in range(self.NG)]
        self.ps_tiles = [st.enter_context(nc.psum_tensor("ps%d" % i, [128, 512], F32)) for i in range(8)]
        self.ps_res = [k.res("ps%d" % i) for i in range(8)]
        self.ps_i = 0

        self.setup_consts()
        self.adaln()
        for b in range(NB):
            self.sequence(b)
        k.final_wait(self.final_res)
        return nc

    NV = 2 * 64 + 8
    NSMALL = 8 + 64 * 6 + 128

    def setup_consts(self):
        k, nc = self.k, self.nc
        self.c_res = k.res("consts")
        cr = self.c_res
        self.ident_f = self.sb([128, 128], F32, "identf")
        self.ident_b = self.sb([128, 128], BF16, "identb")
        self.maskadd = self.sb([128, 128], F32, "maskadd")
        self.mask01 = self.sb([128, 128], BF16, "mask01")
        self.ones_f = self.sb([128, 128], F32, "onesf")
        self.iot = self.sb([128, 128], F32, "iot")
        k.op("pool", lambda h: h.iota(self.iot[:], pattern=[[1, 128]], base=0, channel_multiplier=-1,
                                      allow_small_or_imprecise_dtypes=True), writes=[cr])
        k.op("dve", lambda h: h.tensor_single_scalar(self.ident_f[:], self.iot[:], 0.0, op=ALU.is_equal),
             reads=[cr], writes=[cr])
        k.op("dve", lambda h: h.tensor_copy(self.ident_b[:], self.ident_f[:]), reads=[cr], writes=[cr])
        k.op("dve", lambda h: h.tensor_single_scalar(self.mask01[:], self.iot[:], 0.0, op=ALU.is_ge),
             reads=[cr], writes=[cr])
        k.op("dve", lambda h: h.tensor_scalar(self.maskadd[:], self.iot[:], 0.0, 30000.0,
                                              op0=ALU.is_ge, op1=ALU.mult), reads=[cr], writes=[cr])
        k.op("dve", lambda h: h.tensor_scalar_add(self.maskadd[:], self.maskadd[:], -30000.0),
             reads=[cr], writes=[cr])
        k.op("dve", lambda h: h.memset(self.ones_f[:], 1.0), writes=[cr])
        self.vecT = self.sb([128, self.NV], F32, "vecT")
        self.cT = self.sb([128, KC * self.NB], F32, "cT")
        self.posT = self.sb([128, self.NB * self.NT], I32, "posT")
        self.rbias_b = self.sb([128, NE], F32, "rbias")
        self.small_b = self.sb([128, self.NSMALL], F32, "smallb")
        self.wr_sb = self.sb([128, KC, NE], BF16, "wrouter")
        k.dma("sp", self.vecT[:], self.vecT_d[:, :], writes=[cr], sem_res=cr)
        k.dma("sp", self.cT[:], self.cT_d[:, :], writes=[cr], sem_res=cr)
        k.dma("sp", self.posT[:], self.pos_d[:, :], writes=[cr], sem_res=cr)
        k.dma("sp", self.rbias_b[:], self.rbias_d.partition_broadcast(128), writes=[cr], sem_res=cr)
        k.dma("sp", self.small_b[:], self.small_d.partition_broadcast(128), writes=[cr], sem_res=cr)
        k.dma("pool", self.wr_sb[:], self.w_router_d.rearrange("(kc p) e -> p kc e", p=128),
              writes=[cr], sem_res=cr)
        self.onorm_col = self.sb([128, 1], F32, "onormc")
        k.dma("sp", self.onorm_col[:], self.small_d[0:1, 392:520].rearrange("o (p u) -> (o p) u", u=1),
              writes=[cr], sem_res=cr, allow_slow_non_contiguous=True)
        self.cact = self.sb([128, KC * self.NB], F32, "cact")
        k.op("act", lambda h: h.activation(out=self.cact[:], in_=self.cT[:], func=AF.Silu),
             reads=[cr], writes=[cr])

    def adaln(self):
        k, nc, NB = self.k, self.nc, self.NB
        self.modT = self.sb([128, DEPTH * 48 * NB], F32, "modT")
        self.mod_res = k.res("modT")
        CB = 512
        nblk = 6 * D // CB
        ada_stack = ExitStack()
        wbuf = [self.sb([128, KC, CB], F32, "wada%d" % i, ada_stack) for i in range(2)]
        wres = [k.res("wada%d" % i) for i in range(2)]
        it = 0
        for l in range(DEPTH):
            if l not in self.layers:
                continue
            for blk in range(nblk):
                wb, wr = wbuf[it % 2], wres[it % 2]
                it += 1
                k.dma("sp", wb[:], self.w_ada_d[l, :, blk * CB:(blk + 1) * CB].rearrange("(kc p) n -> p kc n", p=128),
                      writes=[wr], sem_res=wr)
                ps, pr = self.psum()
                for jj in range(CB // 128):
                    j = blk * (CB // 128) + jj
                    for kc in range(KC):
                        k.op("pe", lambda h, jj=jj, kc=kc: h.matmul(
                            ps[:, jj * NB:(jj + 1) * NB], lhsT=wb[:, kc, jj * 128:(jj + 1) * 128],
                            rhs=self.cact[:, kc * NB:(kc + 1) * NB], start=(kc == 0), stop=(kc == KC - 1)),
                            reads=[wr, self.c_res], writes=[pr])
                    col = (l * 48 + j) * NB
                    bcol = l * 64 + j
                    k.op("dve", lambda h, jj=jj, col=col, bcol=bcol: h.tensor_scalar(
                        self.modT[:, col:col + NB], ps[:, jj * NB:(jj + 1) * NB],
                        self.vecT[:, bcol:bcol + 1], None, op0=ALU.add),
                        reads=[pr, self.c_res], writes=[self.mod_res])
        k.barrier()
        ada_stack.close()

    def mod_col(self, l, which, b):
        NB = self.NB
        base = (l * 48 + which * 8) * NB + b
        return self.modT[:, base:base + 7 * NB + 1:NB]

    def sequence(self, b):
        k, nc, NT = self.k, self.nc, self.NT
        for i in range(NT):
            k.dma("sp", self.x_sb[:, i, :], self.x_d[b, i * 128:(i + 1) * 128, :],
                  writes=[self.xr[i]], sem_res=self.xr[0])
        for l in self.layers:
            if self.do_mixer:
                self.norm_to_hT(b, l, 0)
                k.barrier()
                if l % 2 == 0:
                    self.mlstm(b, l)
                else:
                    self.diffattn(b, l)
                k.barrier()
            if self.do_moe:
                self.norm_to_hT(b, l, 1)
                k.barrier()
                self.moe(b, l)
                k.barrier()
        outr = self.k.res("out%d" % b)
        for i in range(NT):
            k.dma("sp", self.out_d[b, i * 128:(i + 1) * 128, :], self.x_sb[:, i, :],
                  reads=[self.xr[i]], writes=[outr], sem_res=outr)
        self.final_res = getattr(self, "final_res", []) + [outr]

    def norm_to_hT(self, b, l, which):
        k, nc, NT, NB = self.k, self.nc, self.NT, self.NB
        with ExitStack() as ls:
            ssq = self.sb([128, NT], F32, "ssq", ls)
            rstd = self.sb([128, NT], F32, "rstd", ls)
            junk = self.sb([128, D], BF16, "junk", ls)
            W = self.sb([128, 8], F32, "W", ls)
            xn = [self.sb([128, D], BF16, "xn", ls) for _ in range(2)]
            r_s = k.res("ssq")
            r_j = k.res("junk")
            r_W = k.res("W")
            r_xn = [k.res("xn0"), k.res("xn1")]
            ncol = l * 64 + 48 + which * 8
            sc = self.mod_col(l, which * 3 + 1, b)
            sh = self.mod_col(l, which * 3 + 0, b)
            k.op("dve", lambda h: h.scalar_tensor_tensor(W[:], sc, 1.0, self.vecT[:, ncol:ncol + 8],
                                                         op0=ALU.add, op1=ALU.mult),
                 reads=[self.mod_res, self.c_res], writes=[r_W])
            for i in range(NT):
                k.op("act", lambda h, i=i: h.activation(out=junk[:], in_=self.x_sb[:, i, :], func=AF.Square,
                                                        accum_out=ssq[:, i:i + 1]),
                     reads=[self.xr[i]], writes=[r_j, r_s])
            k.op("dve", lambda h: h.tensor_scalar(rstd[:], ssq[:], 1.0 / D, EPS, op0=ALU.mult, op1=ALU.add),
                 reads=[r_s], writes=[r_s])
            k.op("act", lambda h: h.activation(out=rstd[:], in_=rstd[:], func=AF.Sqrt), reads=[r_s], writes=[r_s])
            k.op("dve", lambda h: h.reciprocal(rstd[:], rstd[:]), reads=[r_s], writes=[r_s])
            def emit_xn(i):
                xb, xr_ = xn[i % 2], r_xn[i % 2]
                k.op("act", lambda h, i=i, xb=xb: h.activation(out=xb[:], in_=self.x_sb[:, i, :], func=AF.Copy,
                                                               scale=rstd[:, i:i + 1]),
                     reads=[self.xr[i], r_s], writes=[xr_])

            emit_xn(0)
            for i in range(NT):
                xb, xr_ = xn[i % 2], r_xn[i % 2]
                ps, pr = self.psum()
                psb = ps[:].bitcast(BF16)
                for j in range(KC):
                    k.op("pe", lambda h, j=j, xb=xb: h.transpose(psb[:, j * 128:(j + 1) * 128],
                                                                 xb[:, j * 128:(j + 1) * 128], self.ident_b[:]),
                         reads=[xr_, self.c_res], writes=[pr])
                if i + 1 < NT:
                    emit_xn(i + 1)
                g = (i * 128) // self.GW
                for j in range(KC):
                    en = "act" if j % 4 == 3 else "dve"
                    dst = self.hT[:, j, i * 128:(i + 1) * 128]
                    src = psb[:, j * 128:(j + 1) * 128]
                    if en == "dve":
                        k.op("dve", lambda h, dst=dst, src=src, j=j: h.tensor_scalar(
                            dst, src, W[:, j:j + 1], sh[:, j:j + 1], op0=ALU.mult, op1=ALU.add),
                            reads=[pr, r_W, self.mod_res], writes=[self.hTr[g]])
                    else:
                        k.op("act", lambda h, dst=dst, src=src, j=j: h.activation(
                            out=dst, in_=src, func=AF.Identity, scale=W[:, j:j + 1], bias=sh[:, j:j + 1]),
                            reads=[pr, r_W, self.mod_res], writes=[self.hTr[g]])

    def bcast_row(self, col8, dst, dst_res, reads):
        k = self.k
        with ExitStack() as ls:
            dg = [self.sb([128, 128], F32, "diag", ls) for _ in range(2)]
            dr = [k.res("diag0"), k.res("diag1")]
            for j in range(KC):
                d_, r_ = dg[j % 2], dr[j % 2]
                k.op("dve", lambda h, d_=d_, j=j: h.tensor_scalar(d_[:], self.ident_f[:], col8[:, j:j + 1], None,
                                                                  op0=ALU.mult),
                     reads=list(reads) + [self.c_res], writes=[r_])
                ps, pr = self.psum()
                k.op("pe", lambda h, d_=d_: h.matmul(ps[:, 0:128], lhsT=self.ones_f[:], rhs=d_[:],
                                                     start=True, stop=True),
                     reads=[r_, self.c_res], writes=[pr])
                k.op("act", lambda h, j=j: h.activation(out=dst[:, j * 128:(j + 1) * 128], in_=ps[:, 0:128],
                                                        func=AF.Copy),
                     reads=[pr], writes=[dst_res])
            k.barrier()

    def moe(self, b, l):
        k, nc, NT, NG, GW = self.k, self.nc, self.NT, self.NG, self.GW
        TPG = GW // 128
        with ExitStack() as ls:
            G2b = self.sb([128, D], F32, "G2b", ls)
            r_G2 = k.res("G2b")
            self.bcast_row(self.mod_col(l, 5, b), G2b, r_G2, [self.mod_res])
            wgu = [self.sb([128, KC, 2 * DE], BF16, "wgu", ls) for _ in range(2)]
            wdn = [self.sb([128, DE // 128, D], BF16, "wdn", ls) for _ in range(2)]
            r_w = [k.res("wexp0"), k.res("wexp1")]
            def load_w(e):
                bi = e % 2
                k.dma("pool", wgu[bi][:], self.w_gu_d[l, e].rearrange("(kc p) n -> p kc n", p=128),
                      writes=[r_w[bi]], sem_res=r_w[bi])
                k.dma("pool", wdn[bi][:], self.w_dn_d[l, e].rearrange("(kc p) n -> p kc n", p=128),
                      writes=[r_w[bi]], sem_res=r_w[bi])

            load_w(0)
            gates = self.sb([128, NT, NE], F32, "gates", ls)
            r_gates = k.res("gates")
            RT = NT * NE
            r_rt = k.res("router_tmp")
            rt = {nm: self.sb([128, NT, NE], F32, nm, ls) for nm in ("scores", "biased", "b2", "ge", "gs")}
            rs = {nm: self.sb([128, NT, 4], F32, nm, ls) for nm in ("m1", "m2", "gsc", "gsel")}
            r1 = {nm: self.sb([128, NT], F32, nm, ls) for nm in ("gmax", "den")}

            def fl(t):
                return t[:].rearrange("p i e -> p (i e)")

            def v4(t):
                return t[:].rearrange("p i (g e) -> p i g e", g=4)

            def b4(t):
                return t[:].unsqueeze(3).broadcast_to([128, NT, 4, 4])

            ps, pr = self.psum()
            for i in range(NT):
                g = (i * 128) // GW
                for kc in range(KC):
                    k.op("pe", lambda h, kc=kc, i=i: h.matmul(ps[:, i * NE:(i + 1) * NE], lhsT=self.hT[:, kc, i * 128:(i + 1) * 128],
                                                          rhs=self.wr_sb[:, kc, :], start=(kc == 0), stop=(kc == KC - 1)),
                         reads=[self.hTr[g], self.c_res], writes=[pr])
            k.op("act", lambda h: h.activation(out=fl(rt["scores"]), in_=ps[:, 0:RT], func=AF.Sigmoid),
                 reads=[pr], writes=[r_rt])
            dv = lambda fn: k.op("dve", fn, reads=[r_rt, self.c_res], writes=[r_rt])
            dv(lambda h: h.tensor_tensor(rt["biased"][:], rt["scores"][:],
                                         self.rbias_b[:].unsqueeze(1).broadcast_to([128, NT, NE]), op=ALU.add))
            dv(lambda h: h.tensor_reduce(rs["m1"][:], v4(rt["biased"]), axis=AX.X, op=ALU.max))
            dv(lambda h: h.tensor_tensor(v4(rt["ge"]), v4(rt["biased"]), b4(rs["m1"]), op=ALU.is_equal))
            dv(lambda h: h.scalar_tensor_tensor(fl(rt["b2"]), fl(rt["ge"]), -1000.0, fl(rt["biased"]),
                                                op0=ALU.mult, op1=ALU.add))
            dv(lambda h: h.tensor_reduce(rs["m2"][:], v4(rt["b2"]), axis=AX.X, op=ALU.max))
            dv(lambda h: h.tensor_add(rs["gsc"][:], rs["m1"][:], rs["m2"][:]))
            dv(lambda h: h.tensor_reduce(r1["gmax"][:], rs["gsc"][:], axis=AX.X, op=ALU.max))
            dv(lambda h: h.tensor_tensor(rs["gsel"][:], rs["gsc"][:],
                                         r1["gmax"][:].unsqueeze(2).broadcast_to([128, NT, 4]), op=ALU.is_ge))
            dv(lambda h: h.tensor_tensor(v4(rt["ge"]), v4(rt["biased"]), b4(rs["m2"]), op=ALU.is_ge))
            dv(lambda h: h.tensor_tensor(v4(rt["ge"]), v4(rt["ge"]), b4(rs["gsel"]), op=ALU.mult))
            dv(lambda h: h.tensor_mul(fl(rt["gs"]), fl(rt["ge"]), fl(rt["scores"])))
            dv(lambda h: h.tensor_reduce(r1["den"][:], rt["gs"][:], axis=AX.X, op=ALU.add))
            dv(lambda h: h.reciprocal(r1["den"][:], r1["den"][:]))
            k.op("dve", lambda h: h.tensor_tensor(gates[:], rt["gs"][:],
                                                  r1["den"][:].unsqueeze(2).broadcast_to([128, NT, NE]), op=ALU.mult),
                 reads=[r_rt], writes=[r_gates])
            if self.debug and "gates" in self.debug and b == 0:
                dr = k.res("dbg_gates")
                k.dma("sp", self.dbg_d["gates"].rearrange("(i p) e -> p i e", p=128), gates[:],
                      reads=[r_gates], writes=[dr], sem_res=dr)
                self.final_res = getattr(self, "final_res", []) + [dr]
            act = [self.sb([128, DE // 128, GW], BF16, "act", ls) for _ in range(2)]
            r_act = [k.res("act0"), k.res("act1")]
            sg = [self.sb([128, GW], F32, "sg", ls) for _ in range(2)]
            r_sg = [k.res("sg0"), k.res("sg1")]
            tmp = [self.sb([128, 512], F32, "tmp", ls) for _ in range(2)]
            r_tmp = [k.res("tmp0"), k.res("tmp1")]

            cnt = [0]

            def emit_gu(e, g, n_):
                bi = e % 2
                a_, ra_ = act[n_ % 2], r_act[n_ % 2]
                for fc in range(DE // 128):
                    psg, prg = self.psum()
                    psu, pru = self.psum()
                    for kc in range(KC):
                        k.op("pe", lambda h, kc=kc, fc=fc: h.matmul(
                            psg[:, 0:GW], lhsT=wgu[bi][:, kc, fc * 128:(fc + 1) * 128],
                            rhs=self.hT[:, kc, g * GW:(g + 1) * GW], start=(kc == 0), stop=(kc == KC - 1)),
                            reads=[r_w[bi], self.hTr[g]], writes=[prg])
                    for kc in range(KC):
                        k.op("pe", lambda h, kc=kc, fc=fc: h.matmul(
                            psu[:, 0:GW], lhsT=wgu[bi][:, kc, DE + fc * 128:DE + (fc + 1) * 128],
                            rhs=self.hT[:, kc, g * GW:(g + 1) * GW], start=(kc == 0), stop=(kc == KC - 1)),
                            reads=[r_w[bi], self.hTr[g]], writes=[pru])
                    s_, rs_ = sg[cnt[0] % 2], r_sg[cnt[0] % 2]
                    cnt[0] += 1
                    k.op("act", lambda h, s_=s_: h.activation(out=s_[:], in_=psg[:, 0:GW], func=AF.Silu),
                         reads=[prg], writes=[rs_])
                    k.op("dve", lambda h, s_=s_, fc=fc, a_=a_: h.tensor_tensor(a_[:, fc, :], s_[:], psu[:, 0:GW],
                                                                             op=ALU.mult),
                         reads=[rs_, pru], writes=[ra_])

            def emit_down(e, g, n_):
                bi = e % 2
                a_, ra_ = act[n_ % 2], r_act[n_ % 2]
                for tt in range(TPG):
                    i = g * TPG + tt
                    for dh in range(2):
                        pso, pro = self.psum()
                        for fc in range(DE // 128):
                            k.op("pe", lambda h, fc=fc, tt=tt, dh=dh: h.matmul(
                                pso[:], lhsT=a_[:, fc, tt * 128:(tt + 1) * 128],
                                rhs=wdn[bi][:, fc, dh * 512:(dh + 1) * 512],
                                start=(fc == 0), stop=(fc == DE // 128 - 1)),
                                reads=[ra_, r_w[bi]], writes=[pro])
                        t_, rt_ = tmp[cnt[0] % 2], r_tmp[cnt[0] % 2]
                        cnt[0] += 1
                        k.op("dve", lambda h, t_=t_, i=i, dh=dh: h.scalar_tensor_tensor(
                            t_[:], pso[:], gates[:, i, e:e + 1], G2b[:, dh * 512:(dh + 1) * 512],
                            op0=ALU.mult, op1=ALU.mult),
                            reads=[pro, r_gates, r_G2], writes=[rt_])
                        k.op("pool", lambda h, t_=t_, i=i, dh=dh: h.tensor_tensor(
                            self.x_sb[:, i, dh * 512:(dh + 1) * 512], self.x_sb[:, i, dh * 512:(dh + 1) * 512],
                            t_[:], op=ALU.add),
                            reads=[rt_, self.xr[i]], writes=[self.xr[i]])

            steps = [(e, g, e * NG + g) for e in range(NE) for g in range(NG)]
            prev = None
            for (e, g, n_) in steps:
                if g == 0 and e + 1 < NE:
                    if prev is not None:
                        emit_down(*prev)
                        prev = None
                    load_w(e + 1)
                emit_gu(e, g, n_)
                if prev is not None:
                    emit_down(*prev)
                prev = (e, g, n_)
            emit_down(*prev)
            k.barrier()

    def mlstm(self, b, l):
        k, nc, NT, NG, GW, S = self.k, self.nc, self.NT, self.NG, self.GW, self.S
        TPG = GW // 128
        H = A_HEADS
        win = self.a_w_in_d
        with ExitStack() as ls:
            Bg_col = self.sb([128, NT, H], F32, "Bgc", ls)
            G_col = self.sb([128, NT, H], F32, "Gc", ls)
            thr_col = self.sb([128, NT, H], F32, "thrc", ls)
            r_cols = k.res("gcols")
            with ExitStack() as ps_:
                wg = self.sb([128, KC, 8], BF16, "wg", ps_)
                r_wg = k.res("wg")
                k.dma("pool", wg[:], win[:, 3072:3080].rearrange("(kc p) n -> p kc n", p=128),
                      writes=[r_wg], sem_res=r_wg)
                bcol = self.sb([4, 2], F32, "bcol", ps_)
                k.dma("sp", bcol[:], self.small_d[0:1, 0:8].rearrange("o (t h) -> (o h) t", t=2),
                      writes=[r_wg], sem_res=r_wg, allow_slow_non_contiguous=True)
                nbf = self.sb([4, 1], F32, "nbf", ps_)
                k.op("dve", lambda h: h.tensor_scalar(nbf[:], bcol[:, 1:2], -1.0, None, op0=ALU.mult),
                     reads=[r_wg], writes=[r_wg])
                A = self.sb([4, S], F32, "rowA", ps_)
                Bt = self.sb([4, S], F32, "rowB", ps_)
                C = self.sb([4, S], F32, "rowC", ps_)
                ones_r = self.sb([4, S], F32, "rowOnes", ps_)
                r_rows = k.res("rows")
                k.op("dve", lambda h: h.memset(ones_r[:], 1.0), writes=[r_rows])
                for g in range(NG):
                    sl = slice(g * GW, (g + 1) * GW)
                    psi, pri = self.psum()
                    for kc in range(KC):
                        k.op("pe", lambda h, kc=kc: h.matmul(psi[0:4, 0:GW], lhsT=wg[:, kc, 0:4], rhs=self.hT[:, kc, sl],
                                                             start=(kc == 0), stop=(kc == KC - 1)),
                             reads=[r_wg, self.hTr[g]], writes=[pri])
                    psf, prf = self.psum()
                    for kc in range(KC):
                        k.op("pe", lambda h, kc=kc: h.matmul(psf[0:4, 0:GW], lhsT=wg[:, kc, 4:8], rhs=self.hT[:, kc, sl],
                                                             start=(kc == 0), stop=(kc == KC - 1)),
                             reads=[r_wg, self.hTr[g]], writes=[prf])
                    k.op("dve", lambda h: h.tensor_scalar(A[:, sl], psi[0:4, 0:GW], bcol[:, 0:1], None, op0=ALU.add),
                         reads=[pri, r_wg], writes=[r_rows])
                    k.op("act", lambda h: h.activation(out=Bt[:, sl], in_=psf[0:4, 0:GW], func=AF.Exp, scale=-1.0,
                                                       bias=nbf[:, 0:1]),
                         reads=[prf, r_wg], writes=[r_rows])
                k.op("act", lambda h: h.activation(out=Bt[:], in_=Bt[:], func=AF.Ln, bias=1.0),
                     reads=[r_rows], writes=[r_rows])
                k.op("dve", lambda h: h.tensor_tensor_scan(C[:], ones_r[:], Bt[:], 0.0, op0=ALU.mult, op1=ALU.add),
                     reads=[r_rows], writes=[r_rows])
                k.op("dve", lambda h: h.tensor_add(A[:], A[:], C[:]), reads=[r_rows], writes=[r_rows])
                k.op("dve", lambda h: h.tensor_tensor_scan(Bt[:], ones_r[:], A[:], 0.0, op0=ALU.mult, op1=ALU.max),
                     reads=[r_rows], writes=[r_rows])
                k.op("dve", lambda h: h.tensor_sub(C[:], C[:], Bt[:]), reads=[r_rows], writes=[r_rows])
                k.op("act", lambda h: h.activation(out=C[:], in_=C[:], func=AF.Exp), reads=[r_rows], writes=[r_rows])
                for src, dst in ((A, Bg_col), (Bt, G_col), (C, thr_col)):
                    pst, prt = self.psum()
                    for c in range(NT):
                        k.op("pe", lambda h, c=c, src=src: h.transpose(pst[:, c * H:(c + 1) * H],
                                                                       src[:, c * 128:(c + 1) * 128],
                                                                       self.ident_f[0:4, 0:4]),
                             reads=[r_rows, self.c_res], writes=[prt])
                    k.op("dve", lambda h, dst=dst: h.tensor_copy(dst[:].rearrange("p c h -> p (c h)"), pst[:, 0:NT * H]),
                         reads=[prt], writes=[r_cols])
                if self.debug and "gcols" in self.debug and b == 0:
                    dr = k.res("dbg_gcols")
                    for qi, t_ in enumerate((Bg_col, G_col, thr_col)):
                        k.dma("sp", self.dbg_d["gcols"][qi].rearrange("(c p) h -> p c h", p=128), t_[:],
                              reads=[r_cols], writes=[dr], sem_res=dr)
                    self.final_res = getattr(self, "final_res", []) + [dr]
                k.barrier()
            if self.stage <= 1:
                return
            G1b = self.sb([128, D], F32, "G1b", ls)
            r_G1 = k.res("G1b")
            self.bcast_row(self.mod_col(l, 2, b), G1b, r_G1, [self.mod_res])
            wout = self.sb([128, KC, D], BF16, "wout", ls)
            r_wout = k.res("wout")
            k.dma("pool", wout[:], self.a_w_out_d.rearrange("(kc p) n -> p kc n", p=128), writes=[r_wout], sem_res=r_wout)
            yT = self.sb([128, KC, S], BF16, "yT", ls)
            r_yT = [k.res("yT%d" % i) for i in range(NT)]
            wsl = [self.sb([128, KC, 768], BF16, "wsl", ls) for _ in range(2)]
            r_wsl = [k.res("wsl0"), k.res("wsl1")]

            def load_wsl(h_):
                bi = h_ % 2
                for (dst0, n, src0) in ((0, 128, h_ * 128), (128, 128, 512 + h_ * 128),
                                        (256, 256, 1024 + h_ * 256), (512, 256, 2048 + h_ * 256)):
                    k.dma("pool", wsl[bi][:, :, dst0:dst0 + n],
                          win[:, src0:src0 + n].rearrange("(kc p) n -> p kc n", p=128),
                          writes=[r_wsl[bi]], sem_res=r_wsl[bi])

            def two(shape, dt, nm):
                return [self.sb(shape, dt, nm, ls) for _ in range(2)], [k.res(nm + "0"), k.res(nm + "1")]

            qT, r_qT = two([128, GW], BF16, "qT")
            kT, r_kT = two([128, GW], BF16, "kT")
            ktok, r_ktok = two([128, 128], BF16, "ktok")
            vaug, r_vaug = two([128, 257], BF16, "vaug")
            sig, r_sig = two([128, 256], F32, "sig")
            diag, r_diag = two([128, 128], F32, "diag")
            z, r_z = two([128, 128], F32, "z")
            DT, r_DT = two([128, 128], F32, "DT")
            itb, r_itb = two([128, 128], F32, "itb")
            PT, r_PT = two([128, 128], BF16, "PT")
            QpT, r_QpT = two([128, 128], BF16, "QpT")
            kw, r_kw = two([128, 128], BF16, "kw")
            u, r_u = two([128, 256], F32, "u")
            ybf, r_ybf = two([128, 256], BF16, "ybf")
            junk2, r_junk2 = two([128, 256], BF16, "junk2")
            sc1, r_sc1 = two([128, 8], F32, "sc1")
            G0c, r_G0 = two([128, 1], F32, "G0c")
            C_f = self.sb([128, 257], F32, "C_f", ls)
            C_b = self.sb([128, 257], BF16, "C_b", ls)
            r_Cf, r_Cb = k.res("C_f"), k.res("C_b")
            mhalf = self.sb([128, 1], F32, "mhalf", ls)
            r_mh = k.res("mhalf")
            k.op("dve", lambda h: h.memset(mhalf[:], -0.5), writes=[r_mh])
            for i_ in range(2):
                k.op("dve", lambda h, i_=i_: h.memset(vaug[i_][:, 256:257], 1.0), writes=[r_vaug[i_]])
            load_wsl(0)
            SCALE = 128.0 ** -0.5
            if self.stage <= 1.2:
                k.barrier()
                return
            it = 0
            for hd in range(H):
                if hd + 1 < H:
                    load_wsl(hd + 1)
                wb, rw = wsl[hd % 2], r_wsl[hd % 2]
                k.op("dve", lambda h: h.memset(C_f[:], 0.0), writes=[r_Cf])
                k.op("dve", lambda h: h.memset(C_b[:], 0.0), writes=[r_Cb])
                k.op("dve", lambda h: h.memset(G0c[it % 2][:], 0.0), writes=[r_G0[it % 2]])
                for c in range(NT):
                    g = c // TPG
                    cg = c % TPG
                    p = it % 2
                    it += 1
                    tok = slice(c * 128, (c + 1) * 128)
                    tg = slice(cg * 128, (cg + 1) * 128)
                    qTg, rqT, kTg, rkT = qT[g % 2], r_qT[g % 2], kT[g % 2], r_kT[g % 2]
                    if cg == 0:
                        gs = slice(g * GW, (g + 1) * GW)
                        psq, prq = self.psum()
                        for kc in range(KC):
                            k.op("pe", lambda h, kc=kc: h.matmul(psq[:, 0:GW], lhsT=wb[:, kc, 0:128], rhs=self.hT[:, kc, gs],
                                                                 start=(kc == 0), stop=(kc == KC - 1)),
                                 reads=[rw, self.hTr[g]], writes=[prq])
                        psk, prk = self.psum()
                        for kc in range(KC):
                            k.op("pe", lambda h, kc=kc: h.matmul(psk[:, 0:GW], lhsT=wb[:, kc, 128:256], rhs=self.hT[:, kc, gs],
                                                                 start=(kc == 0), stop=(kc == KC - 1)),
                                 reads=[rw, self.hTr[g]], writes=[prk])
                        k.op("act", lambda h: h.activation(out=qTg[:], in_=psq[:, 0:GW], func=AF.Copy),
                             reads=[prq], writes=[rqT])
                        k.op("dve", lambda h: h.tensor_scalar(kTg[:], psk[:, 0:GW], SCALE, None, op0=ALU.mult),
                             reads=[prk], writes=[rkT])
                    if self.stage < 1.7:
                        continue
                    pskv, prkv = self.psum()
                    for kc in range(KC):
                        k.op("pe", lambda h, kc=kc: h.matmul(pskv[:, 0:384], lhsT=self.hT[:, kc, tok], rhs=wb[:, kc, 128:512],
                                                             start=(kc == 0), stop=(kc == KC - 1)),
                             reads=[rw, self.hTr[g]], writes=[prkv])
                    if self.stage < 1.72:
                        continue
                    pso, pro = self.psum()
                    for kc in range(KC):
                        k.op("pe", lambda h, kc=kc: h.matmul(pso[:, 0:256], lhsT=self.hT[:, kc, tok], rhs=wb[:, kc, 512:768],
                                                             start=(kc == 0), stop=(kc == KC - 1)),
                             reads=[rw, self.hTr[g]], writes=[pro])
                    if self.stage < 1.73:
                        continue
                    k.op("act", lambda h: h.activation(out=ktok[p][:], in_=pskv[:, 0:128], func=AF.Copy, scale=SCALE),
                         reads=[prkv], writes=[r_ktok[p]])
                    if self.stage < 1.74:
                        continue
                    k.op("act", lambda h: h.activation(out=vaug[p][:, 0:256], in_=pskv[:, 128:384], func=AF.Copy),
                         reads=[prkv], writes=[r_vaug[p]])
                    if self.stage < 1.75:
                        continue
                    k.op("act", lambda h: h.activation(out=sig[p][:], in_=pso[:, 0:256], func=AF.Exp, scale=-1.0),
                         reads=[pro], writes=[r_sig[p]])
                    k.op("dve", lambda h: h.tensor_scalar_add(sig[p][:], sig[p][:], 1.0), reads=[r_sig[p]], writes=[r_sig[p]])
                    k.op("dve", lambda h: h.reciprocal(sig[p][:], sig[p][:]), reads=[r_sig[p]], writes=[r_sig[p]])
                    if self.stage < 3:
                        continue
                    k.op("dve", lambda h: h.tensor_scalar(diag[p][:], self.ident_f[:], G_col[:, c, hd:hd + 1], None,
                                                          op0=ALU.mult),
                         reads=[r_cols, self.c_res], writes=[r_diag[p]])
                    psG, prG = self.psum()
                    k.op("pe", lambda h: h.matmul(psG[:, 0:128], lhsT=self.ones_f[:], rhs=diag[p][:], start=True, stop=True),
                         reads=[r_diag[p], self.c_res], writes=[prG])
                    k.op("dve", lambda h: h.scalar_tensor_tensor(z[p][:], psG[:, 0:128], -1.0, self.maskadd[:],
                                                                 op0=ALU.mult, op1=ALU.add),
                         reads=[prG, self.c_res], writes=[r_z[p]])
                    k.op("act", lambda h: h.activation(out=DT[p][:], in_=z[p][:], func=AF.Exp,
                                                       bias=Bg_col[:, c, hd:hd + 1]),
                         reads=[r_z[p], r_cols], writes=[r_DT[p]])
                    g0, rg0 = G0c[p], r_G0[p]
                    g0n, rg0n = G0c[1 - p], r_G0[1 - p]
                    k.op("act", lambda h: h.activation(out=itb[p][:], in_=psG[:, 0:128], func=AF.Exp, scale=-1.0,
                                                       bias=g0[:, 0:1]),
                         reads=[prG, rg0], writes=[r_itb[p]])
                    k.op("act", lambda h: h.activation(out=sc1[p][:, 0:1], in_=psG[:, 127:128], func=AF.Exp, scale=-1.0,
                                                       bias=Bg_col[:, c, hd:hd + 1]),
                         reads=[prG, r_cols], writes=[r_sc1[p]])
                    k.op("act", lambda h: h.activation(out=sc1[p][:, 1:2], in_=psG[:, 127:128], func=AF.Exp, scale=-1.0,
                                                       bias=g0[:, 0:1]),
                         reads=[prG, rg0], writes=[r_sc1[p]])
                    k.op("act", lambda h: h.activation(out=g0n[:], in_=psG[:, 127:128], func=AF.Copy),
                         reads=[prG], writes=[rg0n])
                    if self.stage < 4:
                        continue
                    psS, prS = self.psum()
                    k.op("pe", lambda h: h.matmul(psS[:, 0:128], lhsT=kTg[:, tg], rhs=qTg[:, tg], start=True, stop=True),
                         reads=[rkT, rqT], writes=[prS])
                    k.op("dve", lambda h: h.tensor_tensor(PT[p][:], psS[:, 0:128], DT[p][:], op=ALU.mult),
                         reads=[prS, r_DT[p]], writes=[r_PT[p]])
                    k.op("dve", lambda h: h.tensor_tensor(QpT[p][:], qTg[:, tg], itb[p][:], op=ALU.mult),
                         reads=[rqT, r_itb[p]], writes=[r_QpT[p]])
                    if self.stage < 5:
                        continue
                    psN, prN = self.psum()
                    k.op("pe", lambda h: h.matmul(psN[:, 0:257], lhsT=PT[p][:], rhs=vaug[p][:], start=True, stop=False),
                         reads=[r_PT[p], r_vaug[p]], writes=[prN])
                    k.op("pe", lambda h: h.matmul(psN[:, 0:257], lhsT=QpT[p][:], rhs=C_b[:], start=False, stop=True),
                         reads=[r_QpT[p], r_Cb], writes=[prN])
                    k.op("dve", lambda h: h.tensor_reduce(sc1[p][:, 2:3], psN[:, 256:257], axis=AX.X, op=ALU.max,
                                                          apply_absolute_value=True),
                         reads=[prN], writes=[r_sc1[p]])
                    k.op("dve", lambda h: h.tensor_scalar(sc1[p][:, 2:3], sc1[p][:, 2:3], thr_col[:, c, hd:hd + 1], None,
                                                          op0=ALU.max),
                         reads=[r_sc1[p], r_cols], writes=[r_sc1[p]])
                    k.op("dve", lambda h: h.reciprocal(sc1[p][:, 2:3], sc1[p][:, 2:3]), reads=[r_sc1[p]], writes=[r_sc1[p]])
                    k.op("act", lambda h: h.activation(out=u[p][:], in_=psN[:, 0:256], func=AF.Copy, scale=sc1[p][:, 2:3]),
                         reads=[prN, r_sc1[p]], writes=[r_u[p]])
                    k.op("act", lambda h: h.activation(out=junk2[p][:], in_=u[p][:], func=AF.Square,
                                                       accum_out=sc1[p][:, 3:4]),
                         reads=[r_u[p]], writes=[r_junk2[p], r_sc1[p]])
                    k.op("dve", lambda h: h.tensor_scalar(sc1[p][:, 4:5], sc1[p][:, 3:4], 1.0 / 256, EPS,
                                                          op0=ALU.mult, op1=ALU.add),
                         reads=[r_sc1[p]], writes=[r_sc1[p]])
                    k.op("act", lambda h: h.activation(out=sc1[p][:, 4:5], in_=sc1[p][:, 4:5], func=AF.Ln),
                         reads=[r_sc1[p]], writes=[r_sc1[p]])
                    k.op("act", lambda h: h.activation(out=sc1[p][:, 4:5], in_=sc1[p][:, 4:5], func=AF.Exp, scale=-0.5),
                         reads=[r_sc1[p]], writes=[r_sc1[p]])
                    k.op("dve", lambda h: h.scalar_tensor_tensor(ybf[p][:], u[p][:], sc1[p][:, 4:5], sig[p][:],
                                                                 op0=ALU.mult, op1=ALU.mult),
                         reads=[r_u[p], r_sc1[p], r_sig[p]], writes=[r_ybf[p]])
                    psT, prT = self.psum()
                    psTb = psT[:].bitcast(BF16)
                    for j in range(2):
                        k.op("pe", lambda h, j=j: h.transpose(psTb[:, j * 128:(j + 1) * 128], ybf[p][:, j * 128:(j + 1) * 128],
                                                              self.ident_b[:]),
                             reads=[r_ybf[p], self.c_res], writes=[prT])
                    for j in range(2):
                        fcol = 128 + hd * 2 + j
                        k.op("act", lambda h, j=j, fcol=fcol: h.activation(
                            out=yT[:, hd * 2 + j, tok], in_=psTb[:, j * 128:(j + 1) * 128], func=AF.Copy,
                            scale=self.vecT[:, fcol:fcol + 1]),
                            reads=[prT, self.c_res], writes=[r_yT[c]])
                    if self.stage < 6:
                        continue
                    k.op("dve", lambda h: h.tensor_scalar(kw[p][:], ktok[p][:], sc1[p][:, 0:1], None, op0=ALU.mult),
                         reads=[r_ktok[p], r_sc1[p]], writes=[r_kw[p]])
                    psC, prC = self.psum()
                    k.op("pe", lambda h: h.matmul(psC[:, 0:257], lhsT=kw[p][:], rhs=vaug[p][:], start=True, stop=True),
                         reads=[r_kw[p], r_vaug[p]], writes=[prC])
                    k.op("dve", lambda h: h.scalar_tensor_tensor(C_f[:], C_f[:], sc1[p][:, 1:2], psC[:, 0:257],
                                                                 op0=ALU.mult, op1=ALU.add),
                         reads=[r_Cf, r_sc1[p], prC], writes=[r_Cf])
                    k.op("act", lambda h: h.activation(out=C_b[:], in_=C_f[:], func=AF.Copy),
                         reads=[r_Cf], writes=[r_Cb])
            tmp, r_tmp = two([128, 512], F32, "tmpo")
            cnt = 0
            for i in range(NT):
                tok = slice(i * 128, (i + 1) * 128)
                for dh in range(2):
                    pso, pro = self.psum()
                    for j in range(KC):
                        k.op("pe", lambda h, j=j: h.matmul(pso[:], lhsT=yT[:, j, tok], rhs=wout[:, j, dh * 512:(dh + 1) * 512],
                                                           start=(j == 0), stop=(j == KC - 1)),
                             reads=[r_yT[i], r_wout], writes=[pro])
                    t_, rt_ = tmp[cnt % 2], r_tmp[cnt % 2]
                    cnt += 1
                    k.op("dve", lambda h, t_=t_: h.tensor_tensor(t_[:], pso[:], G1b[:, dh * 512:(dh + 1) * 512], op=ALU.mult),
                         reads=[pro, r_G1], writes=[rt_])
                    k.op("pool", lambda h, t_=t_: h.tensor_tensor(
                        self.x_sb[:, i, dh * 512:(dh + 1) * 512], self.x_sb[:, i, dh * 512:(dh + 1) * 512], t_[:], op=ALU.add),
                        reads=[rt_, self.xr[i]], writes=[self.xr[i]])
            k.barrier()

    def diffattn(self, b, l):
        k, nc, NT, S = self.k, self.nc, self.NT, self.S
        win = self.b_w_in_d
        lam_init = 0.8 - 0.6 * math.exp(-0.3 * l)
        PI = math.pi
        with ExitStack() as ls:
            r_pre = k.res("attn_pre")
            lamt = self.sb([128, 8], F32, "lamt", ls)
            prod = self.sb([128, 64], F32, "prod", ls)
            sb_ = self.small_b
            for ci, (o1, o2) in enumerate(((136, 200), (264, 328))):
                k.op("dve", lambda h, o1=o1, o2=o2: h.tensor_mul(prod[:], sb_[:, o1:o1 + 64], sb_[:, o2:o2 + 64]),
                     reads=[self.c_res, r_pre], writes=[r_pre])
                k.op("dve", lambda h, ci=ci: h.tensor_reduce(lamt[:, ci:ci + 1], prod[:], axis=AX.X, op=ALU.add),
                     reads=[r_pre], writes=[r_pre])
            k.op("act", lambda h: h.activation(out=lamt[:, 0:2], in_=lamt[:, 0:2], func=AF.Exp), reads=[r_pre], writes=[r_pre])
            k.op("dve", lambda h: h.scalar_tensor_tensor(lamt[:, 2:3], lamt[:, 1:2], -lam_init, lamt[:, 0:1],
                                                         op0=ALU.add, op1=ALU.subtract),
                 reads=[r_pre], writes=[r_pre])
            cosT = self.sb([128, NT, 8], F32, "cosT", ls)
            sinT = self.sb([128, NT, 8], F32, "sinT", ls)
            with ExitStack() as ps_:
                posf = self.sb([128, NT], F32, "posf", ps_)
                invf = self.sb([128, 8], F32, "invf", ps_)
                ang = self.sb([128, NT, 8], F32, "ang", ps_)
                qi = self.sb([128, NT, 8], I32, "qi", ps_)
                qf = self.sb([128, NT, 8], F32, "qf", ps_)
                mk = self.sb([128, NT, 8], F32, "mk", ps_)
                r2 = self.sb([128, NT, 8], F32, "r2", ps_)
                dv = lambda fn: k.op("dve", fn, reads=[r_pre, self.c_res], writes=[r_pre])
                dv(lambda h: h.tensor_copy(posf[:], self.posT[:, b * NT:(b + 1) * NT]))
                for i in range(8):
                    dv(lambda h, i=i: h.memset(invf[:, i:i + 1], float(np.float32(ROPE_THETA) ** np.float32(-2.0 * i / 16))))
                dv(lambda h: h.tensor_tensor(ang[:], posf[:].unsqueeze(2).broadcast_to([128, NT, 8]),
                                             invf[:].unsqueeze(1).broadcast_to([128, NT, 8]), op=ALU.mult))
                dv(lambda h: h.tensor_scalar(qf[:], ang[:], 1.0 / (2 * PI), None, op0=ALU.mult))
                dv(lambda h: h.tensor_copy(qi[:], qf[:]))
                dv(lambda h: h.tensor_copy(qf[:], qi[:]))
                C1 = 6.28125
                C2 = 2 * PI - C1
                dv(lambda h: h.scalar_tensor_tensor(ang[:], qf[:], -C1, ang[:], op0=ALU.mult, op1=ALU.add))
                dv(lambda h: h.scalar_tensor_tensor(ang[:], qf[:], -C2, ang[:], op0=ALU.mult, op1=ALU.add))

                def fold(t):
                    dv(lambda h: h.tensor_single_scalar(mk[:], t[:], PI, op=ALU.is_gt))
                    dv(lambda h: h.scalar_tensor_tensor(t[:], mk[:], -2 * PI, t[:], op0=ALU.mult, op1=ALU.add))
                    dv(lambda h: h.tensor_single_scalar(mk[:], t[:], -PI, op=ALU.is_lt))
                    dv(lambda h: h.scalar_tensor_tensor(t[:], mk[:], 2 * PI, t[:], op0=ALU.mult, op1=ALU.add))
                    dv(lambda h: h.tensor_scalar(t[:], t[:], -PI, PI, op0=ALU.max, op1=ALU.min))

                fold(ang)
                dv(lambda h: h.tensor_scalar_add(r2[:], ang[:], PI / 2))
                fold(r2)
                k.op("act", lambda h: h.activation(out=sinT[:], in_=ang[:], func=AF.Sin), reads=[r_pre], writes=[r_pre])
                k.op("act", lambda h: h.activation(out=cosT[:], in_=r2[:], func=AF.Sin), reads=[r_pre], writes=[r_pre])
                k.barrier()
            if self.stage <= 1.5:
                return
            G1b = self.sb([128, D], F32, "G1b", ls)
            r_G1 = k.res("G1b")
            self.bcast_row(self.mod_col(l, 2, b), G1b, r_G1, [self.mod_res])
            HH = 4
            qT = self.sb([128, HH, S], BF16, "qT", ls)
            kT = self.sb([128, HH, S], BF16, "kT", ls)
            vaug = self.sb([128, NT, HH, 130], BF16, "vaug", ls)
            r_q = [k.res("qTt%d" % i) for i in range(NT)]
            r_k = [k.res("kTt%d" % i) for i in range(NT)]
            r_v = [k.res("vt%d" % i) for i in range(NT)]

            def two(shape, dt, nm, st_):
                return [self.sb(shape, dt, nm, st_) for _ in range(2)], [k.res(nm + "0"), k.res(nm + "1")]

            for half in range(2):
                with ExitStack() as sa:
                    gains = self.sb([128, 16, 64], F32, "gains", sa)
                    r_gains = k.res("gains")
                    for gi in range(16):
                        off = 8 if gi < 8 else 72
                        k.op("dve", lambda h, gi=gi, off=off: h.tensor_copy(gains[:, gi, :], sb_[:, off:off + 64]),
                             reads=[self.c_res], writes=[r_gains])
                    wqkv = self.sb([128, KC, 1536], BF16, "wqkv", sa)
                    r_w = k.res("wqkv")
                    for pi_ in range(3):
                        k.dma("pool", wqkv[:, :, pi_ * 512:(pi_ + 1) * 512],
                              win[:, pi_ * 1024 + half * 512: pi_ * 1024 + (half + 1) * 512].rearrange("(kc p) n -> p kc n", p=128),
                              writes=[r_w], sem_res=r_w)
                    for i in range(NT):
                        k.op("pool", lambda h, i=i: h.memset(vaug[:, i, :, 128:129], 1.0), writes=[r_v[i]])
                    sq, r_sq = two([128, 16, 64], F32, "sq", sa)
                    qn, r_qn = two([128, 16, 64], F32, "qn", sa)
                    qkb, r_qkb = two([128, 16, 64], BF16, "qkb", sa)
                    st_, r_st = two([128, 32], F32, "stat", sa)
                    tA, r_tA = two([128, 16, 8], F32, "tA", sa)
                    tB, r_tB = two([128, 16, 8], F32, "tB", sa)
                    def tile_gen(i, p):
                        g = (i * 128) // self.GW
                        tok = slice(i * 128, (i + 1) * 128)
                        pss = []
                        for pi_ in range(3):
                            ps, pr = self.psum()
                            for kc in range(KC):
                                k.op("pe", lambda h, kc=kc, ps=ps, pi_=pi_: h.matmul(
                                    ps[:], lhsT=self.hT[:, kc, tok], rhs=wqkv[:, kc, pi_ * 512:(pi_ + 1) * 512],
                                    start=(kc == 0), stop=(kc == KC - 1)),
                                    reads=[r_w, self.hTr[g]], writes=[pr])
                            pss.append((ps, pr))
                        (psq, prq), (psk, prk), (psv, prv) = pss
                        k.op("act", lambda h: h.activation(out=vaug[:, i, :, 0:128], in_=psv[:].rearrange("p (a d) -> p a d", a=HH),
                                                           func=AF.Copy),
                             reads=[prv], writes=[r_v[i]])
                        qflat_f = qn[p][:].rearrange("p a d -> p (a d)")
                        k.op("act", lambda h: h.activation(out=qflat_f[:, 0:512], in_=psq[:], func=AF.Copy),
                             reads=[prq], writes=[r_qn[p]])
                        k.op("act", lambda h: h.activation(out=qflat_f[:, 512:1024], in_=psk[:], func=AF.Copy),
                             reads=[prk], writes=[r_qn[p]])
                        yield
                        k.op("act", lambda h: h.activation(out=sq[p][:], in_=qn[p][:], func=AF.Square),
                             reads=[r_qn[p]], writes=[r_sq[p]])
                        yield
                        k.op("dve", lambda h: h.tensor_reduce(st_[p][:, 0:16], sq[p][:], axis=AX.X, op=ALU.add),
                             reads=[r_sq[p]], writes=[r_st[p]])
                        k.op("dve", lambda h: h.tensor_scalar(st_[p][:, 16:32], st_[p][:, 0:16], 1.0 / 64, EPS,
                                                              op0=ALU.mult, op1=ALU.add), reads=[r_st[p]], writes=[r_st[p]])
                        yield
                        k.op("act", lambda h: h.activation(out=st_[p][:, 16:32], in_=st_[p][:, 16:32], func=AF.Ln),
                             reads=[r_st[p]], writes=[r_st[p]])
                        k.op("act", lambda h: h.activation(out=st_[p][:, 16:32], in_=st_[p][:, 16:32], func=AF.Exp, scale=-0.5),
                             reads=[r_st[p]], writes=[r_st[p]])
                        yield
                        k.op("dve", lambda h: h.tensor_scalar(st_[p][:, 16:24], st_[p][:, 16:24], 0.125, None, op0=ALU.mult),
                             reads=[r_st[p]], writes=[r_st[p]])
                        k.op("dve", lambda h: h.tensor_tensor(
                            qn[p][:], qn[p][:], st_[p][:, 16:32].unsqueeze(2).broadcast_to([128, 16, 64]), op=ALU.mult),
                            reads=[r_qn[p], r_st[p]], writes=[r_qn[p]])
                        yield
                        k.op("pool", lambda h: h.tensor_tensor(qn[p][:], qn[p][:], gains[:], op=ALU.mult),
                             reads=[r_qn[p], r_gains], writes=[r_qn[p]])
                        yield
                        k.op("act", lambda h: h.activation(out=qkb[p][:, :, 16:64], in_=qn[p][:, :, 16:64], func=AF.Copy),
                             reads=[r_qn[p]], writes=[r_qkb[p]])
                        cb = cosT[:, i, :].unsqueeze(1).broadcast_to([128, 16, 8])
                        sbb = sinT[:, i, :].unsqueeze(1).broadcast_to([128, 16, 8])
                        x1 = qn[p][:, :, 0:8]
                        x2 = qn[p][:, :, 8:16]
                        k.op("dve", lambda h: h.tensor_tensor(tA[p][:], x1, cb, op=ALU.mult), reads=[r_qn[p], r_pre], writes=[r_tA[p]])
                        k.op("dve", lambda h: h.tensor_tensor(tB[p][:], x2, sbb, op=ALU.mult), reads=[r_qn[p], r_pre], writes=[r_tB[p]])
                        yield
                        k.op("dve", lambda h: h.tensor_tensor(qkb[p][:, :, 0:8], tA[p][:], tB[p][:], op=ALU.subtract),
                             reads=[r_tA[p], r_tB[p]], writes=[r_qkb[p]])
                        k.op("dve", lambda h: h.tensor_tensor(tA[p][:], x2, cb, op=ALU.mult), reads=[r_qn[p], r_pre], writes=[r_tA[p]])
                        k.op("dve", lambda h: h.tensor_tensor(tB[p][:], x1, sbb, op=ALU.mult), reads=[r_qn[p], r_pre], writes=[r_tB[p]])
                        yield
                        k.op("dve", lambda h: h.tensor_tensor(qkb[p][:, :, 8:16], tA[p][:], tB[p][:], op=ALU.add),
                             reads=[r_tA[p], r_tB[p]], writes=[r_qkb[p]])
                        yield
                        pst, prt = self.psum()
                        pstb = pst[:].bitcast(BF16)
                        qflat = qkb[p][:].rearrange("p a d -> p (a d)")
                        for j in range(8):
                            k.op("pe", lambda h, j=j: h.transpose(pstb[:, j * 128:(j + 1) * 128], qflat[:, j * 128:(j + 1) * 128],
                                                                  self.ident_b[:]),
                                 reads=[r_qkb[p], self.c_res], writes=[prt])
                        for a_ in range(HH):
                            k.op("dve", lambda h, a_=a_: h.tensor_scalar(qT[:, a_, tok], pstb[:, a_ * 128:(a_ + 1) * 128],
                                                                       1.0, None, op0=ALU.mult),
                                 reads=[prt], writes=[r_q[i]])
                            k.op("dve", lambda h, a_=a_: h.tensor_scalar(kT[:, a_, tok], pstb[:, 512 + a_ * 128:512 + (a_ + 1) * 128],
                                                                       1.0, None, op0=ALU.mult),
                                 reads=[prt], writes=[r_k[i]])

                    self.run_interleaved([(i,) for i in range(NT)], 2, tile_gen)
                    k.barrier()
                if self.stage < 3:
                    continue
                with ExitStack() as sbk:
                    oT = self.sb([128, HH, S], BF16, "oT", sbk)
                    r_oT = [k.res("oT%d" % i) for i in range(NT)]
                    wo = self.sb([128, HH, D], BF16, "wo", sbk)
                    r_wo = k.res("wo")
                    k.dma("pool", wo[:], self.b_w_out_d[half * 512:(half + 1) * 512, :].rearrange("(kc p) n -> p kc n", p=128),
                          writes=[r_wo], sem_res=r_wo)
                    W = 4

                    def nset(shape, dt, nm):
                        return [self.sb(shape, dt, nm, sbk) for _ in range(W)], [k.res("%s%d" % (nm, i_)) for i_ in range(W)]

                    pTs, r_pTs = nset([128, NT, 128], BF16, "pT")
                    t0, r_t0 = nset([128, 128], F32, "t0")
                    of, r_of = nset([128, 128], F32, "of")
                    ob, r_ob = nset([128, 128], BF16, "ob")
                    jk, r_jk = nset([128, 128], BF16, "jk")
                    sc, r_sc = nset([128, 8], F32, "sc")

                    def attn_iter(hl, j, p):
                        nkb = j + 1
                        qs = slice(j * 128, (j + 1) * 128)
                        pT, r_pT = pTs[p], r_pTs[p]
                        for m in range(2):
                            rows = slice(m * 64, (m + 1) * 64)
                            for kg in range(0, nkb, 4):
                                n = min(4, nkb - kg)
                                ps, pr = self.psum()
                                for idx in range(n):
                                    kb = kg + idx
                                    k.op("pe", lambda h, idx=idx, kb=kb: h.matmul(
                                        ps[:, idx * 128:(idx + 1) * 128], lhsT=kT[rows, hl, kb * 128:(kb + 1) * 128],
                                        rhs=qT[rows, hl, qs], start=True, stop=True),
                                        reads=[r_k[kb], r_q[j]], writes=[pr])
                                k.op("act", lambda h, kg=kg, n=n: h.activation(
                                    out=pT[:, kg:kg + n, :].rearrange("p a t -> p (a t)"), in_=ps[:, 0:n * 128],
                                    func=AF.Exp), reads=[pr], writes=[r_pT])
                                yield
                            k.op("dve", lambda h: h.tensor_tensor(pT[:, j, :], pT[:, j, :], self.mask01[:], op=ALU.mult),
                                 reads=[r_pT, self.c_res], writes=[r_pT])
                            yield
                            acc, ar = self.psum()
                            for kb in range(nkb):
                                k.op("pe", lambda h, kb=kb: h.matmul(
                                    acc[:, 0:129], lhsT=pT[:, kb, :], rhs=vaug[:, kb, hl, 0:129],
                                    start=(kb == 0), stop=(kb == nkb - 1)),
                                    reads=[r_pT, r_v[kb]], writes=[ar])
                            k.op("dve", lambda h: h.reciprocal(sc[p][:, m:m + 1], acc[:, 128:129]), reads=[ar], writes=[r_sc[p]])
                            if m == 0:
                                k.op("act", lambda h: h.activation(out=t0[p][:], in_=acc[:, 0:128], func=AF.Copy, scale=sc[p][:, 0:1]),
                                     reads=[ar, r_sc[p]], writes=[r_t0[p]])
                            else:
                                k.op("dve", lambda h: h.tensor_scalar(sc[p][:, 1:2], sc[p][:, 1:2], lamt[:, 2:3], None, op0=ALU.mult),
                                     reads=[r_sc[p], r_pre], writes=[r_sc[p]])
                                k.op("dve", lambda h: h.scalar_tensor_tensor(of[p][:], acc[:, 0:128], sc[p][:, 1:2], t0[p][:],
                                                                             op0=ALU.mult, op1=ALU.add),
                                     reads=[ar, r_sc[p], r_t0[p]], writes=[r_of[p]])
                            yield
                        k.op("act", lambda h: h.activation(out=jk[p][:], in_=of[p][:], func=AF.Square, accum_out=sc[p][:, 2:3]),
                             reads=[r_of[p]], writes=[r_jk[p], r_sc[p]])
                        yield
                        k.op("dve", lambda h: h.tensor_scalar(sc[p][:, 3:4], sc[p][:, 2:3], 1.0 / 128, EPS, op0=ALU.mult, op1=ALU.add),
                             reads=[r_sc[p]], writes=[r_sc[p]])
                        yield
                        k.op("act", lambda h: h.activation(out=sc[p][:, 3:4], in_=sc[p][:, 3:4], func=AF.Ln),
                             reads=[r_sc[p]], writes=[r_sc[p]])
                        k.op("act", lambda h: h.activation(out=sc[p][:, 3:4], in_=sc[p][:, 3:4], func=AF.Exp, scale=-0.5),
                             reads=[r_sc[p]], writes=[r_sc[p]])
                        yield
                        k.op("dve", lambda h: h.tensor_scalar(ob[p][:], of[p][:], sc[p][:, 3:4], 1.0 - lam_init,
                                                              op0=ALU.mult, op1=ALU.mult),
                             reads=[r_of[p], r_sc[p]], writes=[r_ob[p]])
                        yield
                        pst, prt = self.psum()
                        pstb = pst[:].bitcast(BF16)
                        k.op("pe", lambda h: h.transpose(pstb[:, 0:128], ob[p][:], self.ident_b[:]),
                             reads=[r_ob[p], self.c_res], writes=[prt])
                        k.op("act", lambda h: h.activation(out=oT[:, hl, qs], in_=pstb[:, 0:128], func=AF.Copy,
                                                           scale=self.onorm_col[:, 0:1]),
                             reads=[prt, self.c_res], writes=[r_oT[j]])

                    work = [(hl, j) for hl in range(HH) for j in reversed(range(NT))]
                    self.run_interleaved(work, W, attn_iter)
                    tmp, r_tmp = two([128, 512], F32, "tmpo", sbk)
                    cnt = 0
                    for i in range(NT):
                        tok = slice(i * 128, (i + 1) * 128)
                        for dh in range(2):
                            pso, pro = self.psum()
                            for jx in range(HH):
                                k.op("pe", lambda h, jx=jx: h.matmul(pso[:], lhsT=oT[:, jx, tok], rhs=wo[:, jx, dh * 512:(dh + 1) * 512],
                                                                     start=(jx == 0), stop=(jx == HH - 1)),
                                     reads=[r_oT[i], r_wo], writes=[pro])
                            t_, rt_ = tmp[cnt % 2], r_tmp[cnt % 2]
                            cnt += 1
                            k.op("dve", lambda h, t_=t_: h.tensor_tensor(t_[:], pso[:], G1b[:, dh * 512:(dh + 1) * 512], op=ALU.mult),
                                 reads=[pro, r_G1], writes=[rt_])
                            k.op("pool", lambda h, t_=t_: h.tensor_tensor(
                                self.x_sb[:, i, dh * 512:(dh + 1) * 512], self.x_sb[:, i, dh * 512:(dh + 1) * 512], t_[:], op=ALU.add),
                                reads=[rt_, self.xr[i]], writes=[self.xr[i]])
                    k.barrier()


def host_inputs(inp, core, NB, S):
    b0 = core * NB
    NT = S // 128
    vec = np.zeros((128, Prog.NV), np.float32)
    for l in range(DEPTH):
        vec[:, l * 64:l * 64 + 48] = inp["b_ada"][l].reshape(48, 128).T
        vec[:, l * 64 + 48:l * 64 + 56] = inp["norm1"][l].reshape(8, 128).T
        vec[:, l * 64 + 56:l * 64 + 64] = inp["norm2"][l].reshape(8, 128).T
    vec[:, 128:136] = inp["a_h_norm"][0].reshape(8, 128).T
    c = inp["c"][b0:b0 + NB]
    cT = np.ascontiguousarray(c.reshape(NB, KC, 128).transpose(2, 1, 0).reshape(128, KC * NB))
    pos = inp["positions"][b0:b0 + NB].reshape(NB, NT, 128)
    posT = np.ascontiguousarray(pos.transpose(2, 0, 1).reshape(128, NB * NT)).astype(np.int32)
    small = np.concatenate([inp["a_b_if"][0], inp["b_q_norm"][0], inp["b_k_norm"][0], inp["b_lam_q1"][0],
                            inp["b_lam_k1"][0], inp["b_lam_q2"][0], inp["b_lam_k2"][0],
                            inp["b_o_norm"][0]]).astype(np.float32)[None, :]
    return {
        "x": np.ascontiguousarray(inp["x"][b0:b0 + NB]),
        "cT": cT, "vecT": vec, "posT": posT,
        "w_ada": inp["w_ada"], "a_w_in": inp["a_w_in"][0], "a_w_out": inp["a_w_out"][0],
        "b_w_in": inp["b_w_in"][0], "b_w_out": inp["b_w_out"][0],
        "w_router": inp["w_router"], "rbias": inp["router_bias"][None, :].astype(np.float32),
        "small": small, "moe_w_gu": inp["moe_w_gu"], "moe_w_down": inp["moe_w_down"],
    }


def kernel(**inputs):
    inp = {k_: np.asarray(v) for k_, v in inputs.items()}
    NB = BATCH // N_CORES
    prog = Prog(S=SEQ, NB=NB)
    nc = prog.build()
    in_maps = [host_inputs(inp, c, NB, SEQ) for c in range(N_CORES)]
    res = run_bass_kernel_spmd(nc, in_maps, core_ids=list(range(N_CORES)))
    out = np.concatenate([r["out"] for r in res.results], axis=0)
    return out.astype(np.float32)
```
